# Optimizing a Trainium2 kernel written in Bass

```python
import math, functools
import jax, jax.numpy as jnp
from jax import lax
import numpy as np

D_MODEL = 1024
BATCH = 1
SEQ = 16384
DEPTH = 2

GRID_W = 64
CTX_LEN = 256
HEAD_DIM = 64
N_FOURIER_GROUPS = 4
FOURIER_GROUP_DIM = 64
FOURIER_WIDTH = N_FOURIER_GROUPS * FOURIER_GROUP_DIM
N_DIFF_HEADS = 6
DIFF_QK_DIM = HEAD_DIM // 2
DIFF_V_DIM = HEAD_DIM
DIFF_WIDTH = N_DIFF_HEADS * DIFF_V_DIM
N_GQA_Q_HEADS = 6
N_GQA_KV_HEADS = 2
GQA_GROUP = N_GQA_Q_HEADS // N_GQA_KV_HEADS
GQA_WIDTH = N_GQA_Q_HEADS * HEAD_DIM
MIX_WIDTH = FOURIER_WIDTH + DIFF_WIDTH + GQA_WIDTH
IN_SPLITS = (FOURIER_WIDTH, 2 * N_DIFF_HEADS * DIFF_QK_DIM, 2 * N_DIFF_HEADS * DIFF_QK_DIM, DIFF_WIDTH, GQA_WIDTH, N_GQA_KV_HEADS * HEAD_DIM, N_GQA_KV_HEADS * HEAD_DIM)
IN_WIDTH = 2048
N_EXPERTS = 32
TOP_K = 4
EXPERT_FF = 1024
SWIGLU_ALPHA = 1.702
SWIGLU_LIMIT = 7.0
EXPERT_BLOCK = 128
Q_BLOCK = 128
ROPE_THETA = 10000.0
NORM_EPS = 1e-6
SUBLN_EPS = 1e-5
DIFF_SCALE = DIFF_QK_DIM ** -0.5
GQA_SCALE = HEAD_DIM ** -0.5

kernel_name = 'hybrid_fourier_diffattn_gqa_moe_prefix_dit'


def rms_norm(x, g, eps=NORM_EPS):
    xf = x.astype(jnp.float32)
    y = xf * lax.rsqrt(jnp.mean(xf * xf, axis=-1, keepdims=True) + eps)
    return (y * g.astype(jnp.float32)).astype(x.dtype)


def modulate(h, shift, scale):
    return h * (1 + scale) + shift


def axial_rope_tables(row, col, rot_dim):
    n = rot_dim // 4
    inv = ROPE_THETA ** (-jnp.arange(n, dtype=jnp.float32) / n)
    ang = jnp.concatenate([row[:, None] * inv, col[:, None] * inv], axis=-1)
    return jnp.cos(ang), jnp.sin(ang)


def apply_rope(x, cos, sin):
    xf = x.astype(jnp.float32)
    half = x.shape[-1] // 2
    x1, x2 = xf[..., :half], xf[..., half:]
    cs, sn = cos[None, :, None, :], sin[None, :, None, :]
    return jnp.concatenate([x1 * cs - x2 * sn, x2 * cs + x1 * sn], axis=-1).astype(x.dtype)


def diff_attention_core(q, k, v, lam, g_subln, lam_init):
    s = jnp.einsum('bqhtd,bkhtd->bhtqk', q, k).astype(jnp.float32) * DIFF_SCALE
    p = jax.nn.softmax(s, axis=-1)
    a = (p[:, :, 0] - lam * p[:, :, 1]).astype(v.dtype)
    o = jnp.einsum('bhqk,bkhe->bqhe', a, v)
    o = rms_norm(o, g_subln, eps=SUBLN_EPS) * (1.0 - lam_init)
    return o.reshape(q.shape[0], q.shape[1], -1)


def gqa_core(q, k, v):
    s = jnp.einsum('bqhgd,bkhd->bhgqk', q, k).astype(jnp.float32) * GQA_SCALE
    p = jax.nn.softmax(s, axis=-1).astype(v.dtype)
    o = jnp.einsum('bhgqk,bkhd->bqhgd', p, v)
    return o.reshape(q.shape[0], q.shape[1], -1)


def sweep_query_blocks(core, q):
    B, L = q.shape[0], q.shape[1]
    nb = L // Q_BLOCK
    qb = jnp.moveaxis(q.reshape((B, nb, Q_BLOCK) + q.shape[2:]), 1, 0)
    ob = lax.map(core, qb)
    return jnp.moveaxis(ob, 0, 1).reshape(B, L, ob.shape[-1])


def fourier_mix(u, w_fnet):
    B, L, _ = u.shape
    ug = u.reshape(B, L, N_FOURIER_GROUPS, FOURIER_GROUP_DIM).astype(jnp.float32)
    f = jnp.fft.fft2(ug, axes=(1, 3), norm='ortho').real.astype(u.dtype)
    y = jnp.einsum('blgc,gce->blge', f, w_fnet)
    return y.reshape(B, L, FOURIER_WIDTH)


def token_mixers(h, hc, need_ctx, w_in, w_out, w_fnet, lam, lam_init, g_subln, g_q, g_k, rope_d, rope_g):
    B, L, _ = h.shape
    C = hc.shape[1]
    pts = [int(v) for v in np.cumsum(IN_SPLITS)[:-1]]
    f, dq, dk, dv, gq, gk, gv = jnp.split(h @ w_in, pts, axis=-1)
    fc, dqc, dkc, dvc, gqc, gkc, gvc = jnp.split(hc @ w_in, pts, axis=-1)
    cos_d, sin_d = rope_d
    dq = apply_rope(dq.reshape(B, L, 2 * N_DIFF_HEADS, DIFF_QK_DIM), cos_d, sin_d).reshape(B, L, N_DIFF_HEADS, 2, DIFF_QK_DIM)
    dk = apply_rope(dk.reshape(B, L, 2 * N_DIFF_HEADS, DIFF_QK_DIM), cos_d, sin_d).reshape(B, L, N_DIFF_HEADS, 2, DIFF_QK_DIM)
    dv = dv.reshape(B, L, N_DIFF_HEADS, DIFF_V_DIM)
    dkc = dkc.reshape(B, C, N_DIFF_HEADS, 2, DIFF_QK_DIM)
    dvc = dvc.reshape(B, C, N_DIFF_HEADS, DIFF_V_DIM)
    dk_all = jnp.concatenate([dkc, dk], axis=1)
    dv_all = jnp.concatenate([dvc, dv], axis=1)
    d_out = sweep_query_blocks(lambda qi: diff_attention_core(qi, dk_all, dv_all, lam, g_subln, lam_init), dq)
    cos_g, sin_g = rope_g
    gq = apply_rope(rms_norm(gq.reshape(B, L, N_GQA_Q_HEADS, HEAD_DIM), g_q), cos_g, sin_g).reshape(B, L, N_GQA_KV_HEADS, GQA_GROUP, HEAD_DIM)
    gk = apply_rope(rms_norm(gk.reshape(B, L, N_GQA_KV_HEADS, HEAD_DIM), g_k), cos_g, sin_g)
    gv = gv.reshape(B, L, N_GQA_KV_HEADS, HEAD_DIM)
    gkc = rms_norm(gkc.reshape(B, C, N_GQA_KV_HEADS, HEAD_DIM), g_k)
    gvc = gvc.reshape(B, C, N_GQA_KV_HEADS, HEAD_DIM)
    gk_all = jnp.concatenate([gkc, gk], axis=1)
    gv_all = jnp.concatenate([gvc, gv], axis=1)
    g_out = sweep_query_blocks(lambda qi: gqa_core(qi, gk_all, gv_all), gq)
    f_out = fourier_mix(f, w_fnet)
    o = jnp.concatenate([f_out, d_out, g_out], axis=-1) @ w_out
    if not need_ctx:
        return o, None
    dqc = dqc.reshape(B, C, N_DIFF_HEADS, 2, DIFF_QK_DIM)
    gqc = rms_norm(gqc.reshape(B, C, N_GQA_Q_HEADS, HEAD_DIM), g_q).reshape(B, C, N_GQA_KV_HEADS, GQA_GROUP, HEAD_DIM)
    d_out_c = diff_attention_core(dqc, dkc, dvc, lam, g_subln, lam_init)
    g_out_c = gqa_core(gqc, gkc, gvc)
    f_out_c = fourier_mix(fc, w_fnet)
    oc = jnp.concatenate([f_out_c, d_out_c, g_out_c], axis=-1) @ w_out
    return o, oc


def moe_ffn(h, w_router, b_router, w_gate_up, b_gate_up, w_down, b_down):
    N, D = h.shape
    NK = N * TOP_K
    logits = (h @ w_router + b_router).astype(jnp.float32)
    top_v, top_i = lax.top_k(logits, TOP_K)
    gates = jax.nn.softmax(top_v, axis=-1)
    flat_e = top_i.reshape(-1).astype(jnp.int32)
    flat_t = jnp.arange(NK, dtype=jnp.int32) // TOP_K
    flat_g = gates.reshape(-1)
    se, order = lax.sort((flat_e, jnp.arange(NK, dtype=jnp.int32)), num_keys=1, is_stable=True)
    counts = jax.ops.segment_sum(jnp.ones_like(flat_e), flat_e, num_segments=N_EXPERTS)
    padded = (counts + EXPERT_BLOCK - 1) // EXPERT_BLOCK * EXPERT_BLOCK
    start_sorted = jnp.cumsum(counts) - counts
    ends_pad = jnp.cumsum(padded)
    start_pad = ends_pad - padded
    dest = start_pad[se] + jnp.arange(NK, dtype=jnp.int32) - start_sorted[se]
    P = (NK + EXPERT_BLOCK - 1) // EXPERT_BLOCK * EXPERT_BLOCK + N_EXPERTS * EXPERT_BLOCK
    nb = P // EXPERT_BLOCK
    buf_t = jnp.zeros((P,), jnp.int32).at[dest].set(flat_t[order])
    buf_g = jnp.zeros((P,), jnp.float32).at[dest].set(flat_g[order])
    blk_e = jnp.minimum(jnp.searchsorted(ends_pad, jnp.arange(nb, dtype=jnp.int32) * EXPERT_BLOCK, side='right'), N_EXPERTS - 1)
    xb = h[buf_t].reshape(nb, EXPERT_BLOCK, D)

    def expert_block(args):
        xi, e = args
        gu = xi @ w_gate_up[e] + b_gate_up[e]
        glu, lin = gu[:, :EXPERT_FF], gu[:, EXPERT_FF:]
        glu = jnp.minimum(glu, SWIGLU_LIMIT)
        lin = jnp.clip(lin, -SWIGLU_LIMIT, SWIGLU_LIMIT)
        act = glu * jax.nn.sigmoid(SWIGLU_ALPHA * glu) * (lin + 1)
        return act @ w_down[e] + b_down[e]

    yb = lax.map(expert_block, (xb, blk_e)).reshape(P, D)
    return jnp.zeros((N, D), h.dtype).at[buf_t].add(yb * buf_g[:, None].astype(yb.dtype))


def setup_inputs(seed: int = 0) -> dict:
    key = jax.random.key(seed)
    ks = jax.random.split(key, 26)
    D = D_MODEL

    def nrm(k, shape, scale):
        return jax.random.normal(k, shape, jnp.float32) * scale

    return {
        'x': nrm(ks[0], (BATCH, SEQ, D), 1.0),
        'c': nrm(ks[1], (BATCH, D), 1.0),
        'ctx': nrm(ks[2], (BATCH, CTX_LEN, D), 1.0),
        'c_ctx': nrm(ks[3], (D,), 1.0),
        'w_mod': nrm(ks[4], (DEPTH, D, 6 * D), 0.5 * D ** -0.5),
        'b_mod': nrm(ks[5], (DEPTH, 6 * D), 0.02),
        'g_norm1': 1.0 + nrm(ks[6], (DEPTH, D), 0.02),
        'g_norm2': 1.0 + nrm(ks[7], (DEPTH, D), 0.02),
        'w_in': nrm(ks[8], (DEPTH, D, IN_WIDTH), D ** -0.5),
        'w_out': nrm(ks[9], (DEPTH, MIX_WIDTH, D), MIX_WIDTH ** -0.5),
        'w_fnet': nrm(ks[10], (DEPTH, N_FOURIER_GROUPS, FOURIER_GROUP_DIM, FOURIER_GROUP_DIM), FOURIER_GROUP_DIM ** -0.5),
        'lambda_q1': nrm(ks[11], (DEPTH, DIFF_QK_DIM), 0.1),
        'lambda_k1': nrm(ks[12], (DEPTH, DIFF_QK_DIM), 0.1),
        'lambda_q2': nrm(ks[13], (DEPTH, DIFF_QK_DIM), 0.1),
        'lambda_k2': nrm(ks[14], (DEPTH, DIFF_QK_DIM), 0.1),
        'g_subln': 1.0 + nrm(ks[15], (DEPTH, DIFF_V_DIM), 0.02),
        'g_qnorm': 1.0 + nrm(ks[16], (DEPTH, HEAD_DIM), 0.02),
        'g_knorm': 1.0 + nrm(ks[17], (DEPTH, HEAD_DIM), 0.02),
        'w_router': nrm(ks[18], (DEPTH, D, N_EXPERTS), D ** -0.5),
        'b_router': nrm(ks[19], (DEPTH, N_EXPERTS), 0.01),
        'w_gate_up': nrm(ks[20], (DEPTH, N_EXPERTS, D, 2 * EXPERT_FF), D ** -0.5),
        'b_gate_up': nrm(ks[21], (DEPTH, N_EXPERTS, 2 * EXPERT_FF), 0.02),
        'w_down': nrm(ks[22], (DEPTH, N_EXPERTS, EXPERT_FF, D), EXPERT_FF ** -0.5),
        'b_down': nrm(ks[23], (DEPTH, N_EXPERTS, D), 0.02),
        'g_final': 1.0 + nrm(ks[24], (D,), 0.02),
    }


def reference(x, c, ctx, c_ctx, w_mod, b_mod, g_norm1, g_norm2, w_in, w_out, w_fnet, lambda_q1, lambda_k1, lambda_q2, lambda_k2, g_subln, g_qnorm, g_knorm, w_router, b_router, w_gate_up, b_gate_up, w_down, b_down, g_final):
    B, L, D = x.shape
    rows = L // GRID_W
    row_ids = jnp.repeat(jnp.arange(rows, dtype=jnp.float32), GRID_W)
    col_ids = jnp.tile(jnp.arange(GRID_W, dtype=jnp.float32), rows)
    rope_d = axial_rope_tables(row_ids, col_ids, DIFF_QK_DIM)
    rope_g = axial_rope_tables(row_ids, col_ids, HEAD_DIM)
    xc = ctx
    for l in range(DEPTH):
        need_ctx = l < DEPTH - 1
        mod = jax.nn.silu(c) @ w_mod[l] + b_mod[l]
        modc = jax.nn.silu(c_ctx) @ w_mod[l] + b_mod[l]
        sh1, sc1, gt1, sh2, sc2, gt2 = jnp.split(mod[:, None, :], 6, axis=-1)
        sh1c, sc1c, gt1c, sh2c, sc2c, gt2c = jnp.split(modc, 6, axis=-1)
        lam_init = 0.8 - 0.6 * math.exp(-0.3 * l)
        lam = (jnp.exp(jnp.sum(lambda_q1[l].astype(jnp.float32) * lambda_k1[l].astype(jnp.float32)))
               - jnp.exp(jnp.sum(lambda_q2[l].astype(jnp.float32) * lambda_k2[l].astype(jnp.float32))) + lam_init)
        h = modulate(rms_norm(x, g_norm1[l]), sh1, sc1)
        hc = modulate(rms_norm(xc, g_norm1[l]), sh1c, sc1c)
        o, oc = token_mixers(h, hc, need_ctx, w_in[l], w_out[l], w_fnet[l], lam, lam_init, g_subln[l], g_qnorm[l], g_knorm[l], rope_d, rope_g)
        x = x + gt1 * o
        h2 = modulate(rms_norm(x, g_norm2[l]), sh2, sc2)
        if need_ctx:
            xc = xc + gt1c * oc
            h2c = modulate(rms_norm(xc, g_norm2[l]), sh2c, sc2c)
            n_ctx = B * xc.shape[1]
            tokens = jnp.concatenate([h2c.reshape(-1, D), h2.reshape(-1, D)], axis=0)
            y = moe_ffn(tokens, w_router[l], b_router[l], w_gate_up[l], b_gate_up[l], w_down[l], b_down[l])
            xc = xc + gt2c * y[:n_ctx].reshape(xc.shape)
            x = x + gt2 * y[n_ctx:].reshape(x.shape)
        else:
            y = moe_ffn(h2.reshape(-1, D), w_router[l], b_router[l], w_gate_up[l], b_gate_up[l], w_down[l], b_down[l])
            x = x + gt2 * y.reshape(x.shape)
    return rms_norm(x, g_final)
```

```python
import contextlib
import math
import numpy as np
import ml_dtypes
import concourse.bass as bass
import concourse.mybir as mybir
from concourse.bass_utils import run_bass_kernel_spmd

F32 = mybir.dt.float32
BF16 = mybir.dt.bfloat16
AF = mybir.ActivationFunctionType
ALU = mybir.AluOpType
AX = mybir.AxisListType
NPBF = ml_dtypes.bfloat16

NCORES = 8
D = 1024
SEQ = 16384
CTX = 256
TLOC = SEQ // NCORES
NT = CTX + TLOC
NTILES = NT // 128
NKEYS = CTX + SEQ
NKT = NKEYS // 128
NEXP = 32
FF = 1024
DEPTH = 2
NORM_EPS = 1e-6
SUBLN_EPS = 1e-5
DIFF_SCALE = 32 ** -0.5
GQA_SCALE = 64 ** -0.5

ENGS = ("pe", "dve", "act", "pool", "sp")


class Rec:
    def __init__(self, nc, same_engine_sync=True):
        self.nc = nc
        self.ops = []
        self.last_w = {}
        self.readers = {}
        self.same_engine_sync = same_engine_sync

    def op(self, eng, fn, reads=(), writes=(), dma_key=None, inc=16):
        i = len(self.ops)
        deps = set()
        for k in reads:
            if k in self.last_w:
                deps.add(self.last_w[k])
        for k in writes:
            if k in self.last_w:
                deps.add(self.last_w[k])
            for r in self.readers.get(k, {}).values():
                deps.add(r)
        self.ops.append(dict(eng=eng, fn=fn, deps=deps, dma_key=dma_key, is_dma=dma_key is not None, inc=inc))
        rk = ("dma", i) if dma_key is not None else eng
        for k in reads:
            self.readers.setdefault(k, {})[rk] = i
        for k in writes:
            self.last_w[k] = i
            self.readers[k] = {}
        return i

    def finalize(self):
        nc = self.nc
        ops = self.ops
        n = len(ops)

        def need_sync(od, o):
            if od["is_dma"] or o["is_dma"]:
                return True
            if od["eng"] != o["eng"]:
                return True
            if od["eng"] == "pe":
                return False
            return self.same_engine_sync

        needed = [False] * n
        for i, o in enumerate(ops):
            for d in o["deps"]:
                if need_sync(ops[d], o):
                    needed[d] = True
            if o["is_dma"]:
                needed[i] = True
        eng_cnt = {e: 0 for e in ENGS}
        dma_cnt = {}
        sig = [None] * n
        for i, o in enumerate(ops):
            if not needed[i]:
                continue
            if o["is_dma"]:
                k = o["dma_key"]
                dma_cnt[k] = dma_cnt.get(k, 0) + o['inc']
                sig[i] = (("dma", k), dma_cnt[k])
            else:
                eng_cnt[o["eng"]] += 1
                sig[i] = (("eng", o["eng"]), eng_cnt[o["eng"]])
        waited = {e: {} for e in ENGS}
        for i, o in enumerate(ops):
            w = {}
            for d in o["deps"]:
                od = ops[d]
                if sig[d] is None or not need_sync(od, o):
                    continue
                sname, val = sig[d]
                if val > w.get(sname, 0):
                    w[sname] = val
            ww = []
            for sname, val in w.items():
                if waited[o["eng"]].get(sname, 0) >= val:
                    continue
                waited[o["eng"]][sname] = val
                ww.append((sname, val))
            o["waits"] = ww
            o["sig"] = sig[i]
        sp_keys = set(o["dma_key"] for o in ops if o["is_dma"] and o["eng"] == "sp")
        final = [(("dma", k), v) for k, v in dma_cnt.items() if k in sp_keys]
        sem_names = [("eng", e) for e in ENGS if eng_cnt[e] > 0] + [("dma", k) for k in dma_cnt]
        self.n_sems = len(sem_names)
        self.max_cnt = dict(eng_cnt)
        with contextlib.ExitStack() as st:
            sems = {}
            for j, sn in enumerate(sem_names):
                sems[sn] = st.enter_context(nc.semaphore("s%d" % j))
            block = st.enter_context(nc.Block())
            per_eng = {e: [o for o in ops if o["eng"] == e] for e in ENGS}

            def run(eng_name, eng, extra_final=False):
                for o in per_eng[eng_name]:
                    for sn, val in o["waits"]:
                        eng.wait_ge(sems[sn], val)
                    ins = o["fn"](eng)
                    if o["sig"] is not None:
                        if o["is_dma"] and o["inc"] == 1:
                            ins.then_inc(sems[o["sig"][0]])
                        else:
                            ins.then_inc(sems[o["sig"][0]], o["inc"] if o["is_dma"] else 1)
                if extra_final:
                    for sn, val in final:
                        eng.wait_ge(sems[sn], val)

            @block.sync
            def _(e):
                run("sp", e, extra_final=True)

            @block.tensor
            def _(e):
                run("pe", e)

            @block.vector
            def _(e):
                run("dve", e)

            @block.scalar
            def _(e):
                run("act", e)

            @block.gpsimd
            def _(e):
                run("pool", e)


class KB:
    def __init__(self):
        self.nc = bass.Bass("TRN2", target_bir_lowering=False)
        self.st = contextlib.ExitStack()
        self.R = Rec(self.nc)
        self.in_names = []
        self.out_names = []

    def din(self, name, shape, dt):
        self.in_names.append(name)
        return self.nc.dram_tensor(name, list(shape), dt, kind="ExternalInput").ap()

    def dout(self, name, shape, dt):
        self.out_names.append(name)
        return self.nc.dram_tensor(name, list(shape), dt, kind="ExternalOutput").ap()

    def sb(self, name, shape, dt):
        return self.st.enter_context(self.nc.sbuf_tensor("sb_" + name, list(shape), dt))

    def ps(self, name, shape, dt):
        return self.st.enter_context(self.nc.psum_tensor("pp_" + name, list(shape), dt))

    def op(self, eng, fn, reads=(), writes=(), dma_key=None, inc=16):
        return self.R.op(eng, fn, reads, writes, dma_key, inc)

    def finish(self):
        self.R.finalize()
        self.st.close()
        return self.nc


def make_identities(k):
    identf = k.sb("identf", [128, 128], F32)
    identb = k.sb("identb", [128, 128], BF16)
    k.op("pool", lambda e: e.memset(identf[:], 1.0), writes=["identf"])
    k.op("pool", lambda e: e.affine_select(out=identf[:], in_=identf[:], pattern=[[-1, 128]], compare_op=ALU.is_equal,
                                           fill=0.0, base=0, channel_multiplier=1), reads=["identf"], writes=["identf"])
    k.op("dve", lambda e: e.tensor_copy(out=identb[:], in_=identf[:]), reads=["identf"], writes=["identb"])
    return identf, identb


def emit_mod(k, csil_d, wmod_d, bmod_d, blocks, mod_lat, mod_ctx, ps_mod, pskey, nbuf=2):
    csil = k.sb("csil", [128, 16], F32)
    ones = k.sb("modones", [128, 128], F32)
    rep = k.sb("modrep", [128, 16, 128], F32)
    wbuf = [k.sb("modw%d" % i, [128, 8, 256], F32) for i in range(nbuf)]
    bbuf = [k.sb("modb%d" % i, [128, 256], F32) for i in range(nbuf)]
    k.op("sp", lambda e: e.dma_start(out=csil[:], in_=csil_d[:, :]), writes=["csil"], dma_key="csil")
    k.op("act", lambda e: e.activation(out=csil[:], in_=csil[:], func=AF.Silu), reads=["csil"], writes=["csil"])
    k.op("pool", lambda e: e.memset(ones[:], 1.0), writes=["modones"])
    for j in range(16):
        k.op("dve", lambda e, j=j: e.tensor_scalar(out=rep[:, j, :], in0=ones[:], scalar1=csil[:, j:j + 1], scalar2=None,
                                                   op0=ALU.mult), reads=["csil", "modones"], writes=["modrep"])
    sub = []
    for bi, cb in enumerate(blocks):
        sub.append((bi * 512, cb * 512))
        sub.append((bi * 512 + 256, cb * 512 + 256))
    for bi, (lo, go) in enumerate(sub):
        s = bi % nbuf
        k.op("sp", lambda e, s=s, go=go: e.dma_start(
            out=wbuf[s][:], in_=wmod_d[:, go:go + 256].rearrange("(k p) n -> p k n", p=128)),
            writes=["modw%d" % s], dma_key="modw%d" % s)
        k.op("sp", lambda e, s=s, go=go: e.dma_start(out=bbuf[s][:], in_=bmod_d[:, go:go + 256]),
             writes=["modb%d" % s], dma_key="modb%d" % s)
        for si, dst in enumerate((mod_lat, mod_ctx)):
            for kk in range(8):
                k.op("pe", lambda e, s=s, kk=kk, si=si: e.matmul(ps_mod[:, 0:256], lhsT=rep[:, si * 8 + kk, :], rhs=wbuf[s][:, kk, :],
                                                                 start=(kk == 0), stop=(kk == 7)),
                     reads=["modrep", "modw%d" % s], writes=[pskey])
            k.op("dve", lambda e, s=s, lo=lo, dst=dst: e.tensor_tensor(out=dst[:, lo:lo + 256], in0=ps_mod[:, 0:256],
                                                                        in1=bbuf[s][:], op=ALU.add),
                 reads=[pskey, "modb%d" % s], writes=["mod"])


def build_A(layer_has_rope_ctx=False):
    k = KB()
    xin = k.din("xin", [NT, D], F32)
    csil_d = k.din("csil", [128, 16], F32)
    wmod_d = k.din("wmod", [D, 2048], F32)
    bmod_d = k.din("bmod", [128, 2048], F32)
    g1_d = k.din("g1", [128, D], F32)
    win_d = k.din("win", [D, 2048], F32)
    gqk_d = k.din("gqk", [128, 512], F32)
    rope_d = k.din("rope", [TLOC // 128, 128, 896], F32)
    qkT_o = k.dout("qkT", [10, 128, NT], BF16)
    vf_o = k.dout("vf", [NT, 768], BF16)

    identf, identb = make_identities(k)
    ps_big = k.ps("ps_big", [128, 2048], F32)
    ps_t = k.ps("ps_t", [128, 1024], BF16)
    ps_t2 = k.ps("ps_t2", [128, 2048], BF16)
    ps_m = k.ps("ps_m", [128, 512], F32)
    mod_lat = k.sb("mod_lat", [128, 2048], F32)
    mod_ctx = k.sb("mod_ctx", [128, 2048], F32)
    emit_mod(k, csil_d, wmod_d, bmod_d, [0, 1, 2, 3], mod_lat, mod_ctx, ps_m, "ps_m")
    g1 = k.sb("g1", [128, D], F32)
    gqk = k.sb("gqk", [128, 512], F32)
    k.op("sp", lambda e: e.dma_start(out=g1[:], in_=g1_d[:, :]), writes=["g1"], dma_key="g1")
    k.op("sp", lambda e: e.dma_start(out=gqk[:], in_=gqk_d[:, :]), writes=["gqk"], dma_key="gqk")
    A1 = [k.sb("A1_%d" % i, [128, D], F32) for i in range(2)]
    mods = (mod_lat, mod_ctx)
    for i in range(2):
        k.op("dve", lambda e, i=i: e.scalar_tensor_tensor(out=A1[i][:], in0=mods[i][:, 1024:2048], scalar=1.0, in1=g1[:],
                                                           op0=ALU.add, op1=ALU.mult), reads=["mod", "g1"], writes=["A1"])
    winb = k.sb("winb", [128, 8, 2048], BF16)
    for kk in range(8):
        k.op("pool", lambda e, kk=kk: e.dma_start(out=winb[:, kk, :], in_=win_d[kk * 128:(kk + 1) * 128, :]),
             writes=["winb"], dma_key="winb")
    epsn = k.sb("epsn", [128, 1], F32)
    epsq = k.sb("epsq", [128, 1], F32)
    k.op("pool", lambda e: e.memset(epsn[:], NORM_EPS), writes=["eps"])
    k.op("pool", lambda e: e.memset(epsq[:], NORM_EPS), writes=["eps"])
    ss = k.sb("ss", [128, NTILES], F32)
    rstd = k.sb("rstd", [128, NTILES], F32)
    k.op("pool", lambda e: e.memset(ss[:], 0.0), writes=["ss"])
    xt = [k.sb("xt%d" % i, [128, D], F32) for i in range(2)]
    rp = [k.sb("rp%d" % i, [128, 896], F32) for i in range(2)]
    junk = k.sb("junk", [128, D], F32)
    hn = k.sb("hn", [128, D], F32)
    hnb = k.sb("hnb", [128, D], BF16)
    hnT = k.sb("hnT", [128, D], BF16)
    vf = [k.sb("vf%d" % i, [128, 768], BF16) for i in range(2)]
    qk = k.sb("qk", [128, 1280], BF16)
    qkT = [k.sb("qkT%d" % i, [128, 1280], BF16) for i in range(2)]
    t1 = k.sb("t1", [128, 384], F32)
    t2 = k.sb("t2", [128, 384], F32)
    t3 = k.sb("t3", [128, 384], F32)
    t4 = k.sb("t4", [128, 384], F32)
    sq = k.sb("sq", [128, 512], F32)
    ssh = k.sb("ssh", [128, 8], F32)
    rsh = k.sb("rsh", [128, 8], F32)
    nrm = k.sb("nrm", [128, 512], F32)
    nrm2 = k.sb("nrm2", [128, 512], F32)

    for t in range(NTILES):
        s = t % 2
        is_ctx = t < 2
        mi = 1 if is_ctx else 0
        xk, rk, vk, qtk = "xt%d" % s, "rp%d" % s, "vf%d" % s, "qkT%d" % s
        k.op("sp", lambda e, s=s, t=t: e.dma_start(out=xt[s][:], in_=xin[t * 128:(t + 1) * 128, :]), writes=[xk], dma_key=xk)
        if not is_ctx:
            k.op("sp", lambda e, s=s, t=t: e.dma_start(out=rp[s][:], in_=rope_d[t - 2, :, :]), writes=[rk], dma_key=rk)
        k.op("act", lambda e, s=s, t=t: e.activation(out=junk[:], in_=xt[s][:], func=AF.Square, accum_out=ss[:, t:t + 1]),
             reads=[xk, "ss"], writes=["junk", "ss"])
        k.op("act", lambda e, t=t: e.activation(out=rstd[:, t:t + 1], in_=ss[:, t:t + 1], func=AF.Sqrt, bias=epsn[:], scale=1.0 / D),
             reads=["ss", "eps"], writes=["rstd"])
        k.op("dve", lambda e, t=t: e.reciprocal(out=rstd[:, t:t + 1], in_=rstd[:, t:t + 1]), reads=["rstd"], writes=["rstd"])
        k.op("dve", lambda e, s=s, t=t, mi=mi: e.scalar_tensor_tensor(out=hn[:], in0=xt[s][:], scalar=rstd[:, t:t + 1], in1=A1[mi][:],
                                                                      op0=ALU.mult, op1=ALU.mult),
             reads=[xk, "rstd", "A1"], writes=["hn"])
        k.op("pool", lambda e, mi=mi: e.tensor_tensor(out=hnb[:], in0=hn[:], in1=mods[mi][:, 0:1024], op=ALU.add),
             reads=["hn", "mod"], writes=["hnb"])
        for j in range(8):
            k.op("pe", lambda e, j=j: e.transpose(out=ps_t[:, j * 128:(j + 1) * 128], in_=hnb[:, j * 128:(j + 1) * 128], identity=identb[:]),
                 reads=["hnb", "identb"], writes=["ps_t"])
        k.op("act", lambda e: e.activation(out=hnT[:], in_=ps_t[:], func=AF.Copy), reads=["ps_t"], writes=["hnT"])
        for cb in range(4):
            for kk in range(8):
                k.op("pe", lambda e, cb=cb, kk=kk: e.matmul(ps_big[:, cb * 512:(cb + 1) * 512], lhsT=hnT[:, kk * 128:(kk + 1) * 128],
                                                            rhs=winb[:, kk, cb * 512:(cb + 1) * 512], start=(kk == 0), stop=(kk == 7)),
                     reads=["hnT", "winb"], writes=["ps_big"])
        k.op("act", lambda e, s=s: e.activation(out=vf[s][:, 0:384], in_=ps_big[:, 1024:1408], func=AF.Copy), reads=["ps_big"], writes=[vk])
        k.op("act", lambda e, s=s: e.activation(out=vf[s][:, 384:512], in_=ps_big[:, 1920:2048], func=AF.Copy), reads=["ps_big"], writes=[vk])
        k.op("act", lambda e, s=s: e.activation(out=vf[s][:, 512:768], in_=ps_big[:, 0:256], func=AF.Copy), reads=["ps_big"], writes=[vk])
        k.op("sp", lambda e, s=s, t=t: e.dma_start(out=vf_o[t * 128:(t + 1) * 128, :], in_=vf[s][:]), reads=[vk], dma_key=vk + "o")
        if is_ctx:
            k.op("dve", lambda e: e.tensor_copy(out=qk[:, 0:768], in_=ps_big[:, 256:1024]), reads=["ps_big"], writes=["qk"])
        else:
            def v4(ap, h, d):
                return ap.rearrange("p (h t d) -> p h t d", h=h, t=2, d=d)
            for half in range(2):
                src = v4(ps_big[:, 256 + half * 384:256 + (half + 1) * 384], 12, 16)
                dst = v4(qk[:, half * 384:(half + 1) * 384], 12, 16)
                cosd = rp[s][:, 0:192].rearrange("p (h d) -> p h d", h=12)
                sind = rp[s][:, 192:384].rearrange("p (h d) -> p h d", h=12)
                a1 = t1[:, 0:192].rearrange("p (h d) -> p h d", h=12)
                a2 = t2[:, 0:192].rearrange("p (h d) -> p h d", h=12)
                a3 = t3[:, 0:192].rearrange("p (h d) -> p h d", h=12)
                a4 = t4[:, 0:192].rearrange("p (h d) -> p h d", h=12)
                k.op("dve", lambda e, src=src, cosd=cosd, a1=a1: e.tensor_tensor(out=a1, in0=src[:, :, 0, :], in1=cosd, op=ALU.mult),
                     reads=["ps_big", rk], writes=["t1"])
                k.op("dve", lambda e, src=src, sind=sind, a2=a2: e.tensor_tensor(out=a2, in0=src[:, :, 1, :], in1=sind, op=ALU.mult),
                     reads=["ps_big", rk], writes=["t2"])
                k.op("dve", lambda e, src=src, cosd=cosd, a3=a3: e.tensor_tensor(out=a3, in0=src[:, :, 1, :], in1=cosd, op=ALU.mult),
                     reads=["ps_big", rk], writes=["t3"])
                k.op("dve", lambda e, src=src, sind=sind, a4=a4: e.tensor_tensor(out=a4, in0=src[:, :, 0, :], in1=sind, op=ALU.mult),
                     reads=["ps_big", rk], writes=["t4"])
                k.op("pool", lambda e, dst=dst, a1=a1, a2=a2: e.tensor_tensor(out=dst[:, :, 0, :], in0=a1, in1=a2, op=ALU.subtract),
                     reads=["t1", "t2"], writes=["qk"])
                k.op("pool", lambda e, dst=dst, a3=a3, a4=a4: e.tensor_tensor(out=dst[:, :, 1, :], in0=a3, in1=a4, op=ALU.add),
                     reads=["t3", "t4"], writes=["qk"])
        k.op("act", lambda e: e.activation(out=sq[:], in_=ps_big[:, 1408:1920], func=AF.Square), reads=["ps_big"], writes=["sq"])
        k.op("dve", lambda e: e.tensor_reduce(out=ssh[:], in_=sq[:].rearrange("p (h d) -> p h d", h=8), axis=AX.X, op=ALU.add),
             reads=["sq"], writes=["ssh"])
        k.op("act", lambda e: e.activation(out=rsh[:], in_=ssh[:], func=AF.Sqrt, bias=epsq[:], scale=1.0 / 64), reads=["ssh", "eps"], writes=["rsh"])
        k.op("dve", lambda e: e.reciprocal(out=rsh[:], in_=rsh[:]), reads=["rsh"], writes=["rsh"])
        k.op("dve", lambda e: e.tensor_tensor(out=nrm[:].rearrange("p (h d) -> p h d", h=8),
                                              in0=ps_big[:, 1408:1920].rearrange("p (h d) -> p h d", h=8),
                                              in1=rsh[:].unsqueeze(2).to_broadcast([128, 8, 64]), op=ALU.mult),
             reads=["ps_big", "rsh"], writes=["nrm"])
        if is_ctx:
            k.op("pool", lambda e: e.tensor_tensor(out=qk[:, 768:1280], in0=nrm[:], in1=gqk[:], op=ALU.mult),
                 reads=["nrm", "gqk"], writes=["qk"])
        else:
            k.op("pool", lambda e: e.tensor_tensor(out=nrm2[:], in0=nrm[:], in1=gqk[:], op=ALU.mult), reads=["nrm", "gqk"], writes=["nrm2"])
            src = nrm2[:].rearrange("p (h t d) -> p h t d", h=8, t=2, d=32)
            dst = qk[:, 768:1280].rearrange("p (h t d) -> p h t d", h=8, t=2, d=32)
            cosg = rp[s][:, 384:640].rearrange("p (h d) -> p h d", h=8)
            sing = rp[s][:, 640:896].rearrange("p (h d) -> p h d", h=8)
            a1 = t1[:, 0:256].rearrange("p (h d) -> p h d", h=8)
            a2 = t2[:, 0:256].rearrange("p (h d) -> p h d", h=8)
            a3 = t3[:, 0:256].rearrange("p (h d) -> p h d", h=8)
            a4 = t4[:, 0:256].rearrange("p (h d) -> p h d", h=8)
            k.op("dve", lambda e, src=src, cosg=cosg, a1=a1: e.tensor_tensor(out=a1, in0=src[:, :, 0, :], in1=cosg, op=ALU.mult),
                 reads=["nrm2", rk], writes=["t1"])
            k.op("dve", lambda e, src=src, sing=sing, a2=a2: e.tensor_tensor(out=a2, in0=src[:, :, 1, :], in1=sing, op=ALU.mult),
                 reads=["nrm2", rk], writes=["t2"])
            k.op("pool", lambda e, src=src, cosg=cosg, a3=a3: e.tensor_tensor(out=a3, in0=src[:, :, 1, :], in1=cosg, op=ALU.mult),
                 reads=["nrm2", rk], writes=["t3"])
            k.op("pool", lambda e, src=src, sing=sing, a4=a4: e.tensor_tensor(out=a4, in0=src[:, :, 0, :], in1=sing, op=ALU.mult),
                 reads=["nrm2", rk], writes=["t4"])
            k.op("dve", lambda e, dst=dst, a1=a1, a2=a2: e.tensor_tensor(out=dst[:, :, 0, :], in0=a1, in1=a2, op=ALU.subtract),
                 reads=["t1", "t2"], writes=["qk"])
            k.op("pool", lambda e, dst=dst, a3=a3, a4=a4: e.tensor_tensor(out=dst[:, :, 1, :], in0=a3, in1=a4, op=ALU.add),
                 reads=["t3", "t4"], writes=["qk"])
        for j in range(10):
            k.op("pe", lambda e, j=j: e.transpose(out=ps_t2[:, j * 128:(j + 1) * 128], in_=qk[:, j * 128:(j + 1) * 128], identity=identb[:]),
                 reads=["qk", "identb"], writes=["ps_t2"])
        k.op("act", lambda e, s=s: e.activation(out=qkT[s][:], in_=ps_t2[:, 0:1280], func=AF.Copy), reads=["ps_t2"], writes=[qtk])
        k.op("sp", lambda e, s=s, t=t: e.dma_start(out=qkT_o[:, :, t * 128:(t + 1) * 128].rearrange("j p t -> p j t"),
                                                   in_=qkT[s][:].rearrange("p (j t) -> p j t", j=10)),
             reads=[qtk], dma_key=qtk + "o")
    return k


def rope_tables():
    t = np.arange(SEQ)
    row = (t // 64).astype(np.float32)
    col = (t % 64).astype(np.float32)

    def tab(rot_dim):
        n = rot_dim // 4
        inv = (10000.0 ** (-np.arange(n, dtype=np.float32) / n)).astype(np.float32)
        ang = np.concatenate([row[:, None] * inv, col[:, None] * inv], axis=-1).astype(np.float32)
        return np.cos(ang).astype(np.float32), np.sin(ang).astype(np.float32)

    cd, sd = tab(32)
    cg, sg = tab(64)
    full = np.concatenate([np.tile(cd, (1, 12)), np.tile(sd, (1, 12)), np.tile(cg, (1, 8)), np.tile(sg, (1, 8))], axis=1)
    return np.ascontiguousarray(full.reshape(NCORES, TLOC // 128, 128, 896))


def rep128(v):
    v = np.asarray(v, np.float32).reshape(1, -1)
    return np.ascontiguousarray(np.broadcast_to(v, (128, v.shape[1])))


def csil_layout(c, c_ctx):
    a = np.asarray(c, np.float32).reshape(8, 128).T
    b = np.asarray(c_ctx, np.float32).reshape(8, 128).T
    return np.ascontiguousarray(np.concatenate([a, b], axis=1))


def run_A(x_shards, xc, inputs, l, rope):
    nc = _prog("A", build_A)
    common = dict(
        csil=csil_layout(inputs["c"], inputs["c_ctx"]),
        wmod=np.ascontiguousarray(inputs["w_mod"][l][:, 0:2048]),
        bmod=rep128(inputs["b_mod"][l][0:2048]),
        g1=rep128(inputs["g_norm1"][l]),
        win=np.ascontiguousarray(inputs["w_in"][l]),
        gqk=rep128(np.concatenate([np.tile(inputs["g_qnorm"][l], 6), np.tile(inputs["g_knorm"][l], 2)])),
    )
    in_maps = []
    for c in range(NCORES):
        m = dict(common)
        m["xin"] = np.ascontiguousarray(np.concatenate([xc, x_shards[c]], axis=0))
        m["rope"] = rope[c]
        in_maps.append(m)
    res = run_bass_kernel_spmd(nc, in_maps, core_ids=list(range(NCORES)))
    return res.results


def build_B(layer, has_ctx, dbg=None):
    dbg = dbg or {}
    lam_init = 0.8 - 0.6 * math.exp(-0.3 * layer)
    k = KB()
    qT_d = k.din("qT", [6, 128, NT], BF16)
    kTd_d = k.din("kTd", [3, 128, NKEYS], BF16)
    kTg_d = k.din("kTg", [2, 128, NKEYS], BF16)
    vaug_d = k.din("vaug", [8, 128, NKT * 65], BF16)
    fall_d = k.din("fall", [32, 128, 4 * 256], BF16)
    fctx_d = k.din("fctx", [128, 2 * 256], BF16)
    ab_d = k.din("ab", [32, 4 * 1024], BF16)
    dbase_d = k.din("dbase", [128, 4 * 1024], BF16)
    tabCc_d = k.din("tabCc", [128, 2 * 256], BF16)
    tabSc_d = k.din("tabSc", [128, 2 * 256], BF16)
    xin = k.din("xin", [NT, D], F32)
    csil_d = k.din("csil", [128, 16], F32)
    wmod_d = k.din("wmod", [D, D], F32)
    bmod_d = k.din("bmod", [128, D], F32)
    wout_d = k.din("wout", [D, D], F32)
    lam_d = k.din("lam", [128, 128], F32)
    gsub_d = k.din("gsub", [128, 1], F32)
    wf_d = k.din("wf", [2, 128, 64], F32)
    dftc_d = k.din("dftc", [128, 128], F32)
    dfts_d = k.din("dfts", [128, 128], F32)
    x1_o = k.dout("x1", [NT, D], F32)

    identf, identb = make_identities(k)
    psA = k.ps("psA", [128, 1024], F32)
    psB = k.ps("psB", [128, 1024], F32)
    psC = k.ps("psC", [128, 1024], F32)
    psD = k.ps("psD", [128, 1024], F32)
    Sb = [psA[:, 0:512], psA[:, 512:1024], psB[:, 0:512]]
    Ob = [psC[:, 0:512], psC[:, 512:1024], psB[:, 512:1024], psD[:, 512:1024]]
    Mb = psD[:, 0:512]

    mod_lat = k.sb("mod_lat", [128, 1024], F32)
    mod_ctx = k.sb("mod_ctx", [128, 1024], F32)
    mods = (mod_lat, mod_ctx)
    emit_mod(k, csil_d, wmod_d, bmod_d, [0, 1], mod_lat, mod_ctx, psD, "M")
    onesf = k.sb("onesf", [128, 128], F32)
    k.op("pool", lambda e: e.memset(onesf[:], 1.0), writes=["onesf"])
    epsn = k.sb("epsn", [128, 1], F32)
    epss = k.sb("epss", [128, 1], F32)
    k.op("pool", lambda e: e.memset(epsn[:], NORM_EPS), writes=["eps"])
    k.op("pool", lambda e: e.memset(epss[:], SUBLN_EPS), writes=["eps"])
    lamin = k.sb("lamin", [128, 128], F32)
    lamj = k.sb("lamj", [128, 32], F32)
    lams = k.sb("lams", [128, 4], F32)
    neglam = k.sb("neglam", [128, 1], F32)
    gsub = k.sb("gsub", [128, 1], F32)
    k.op("sp", lambda e: e.dma_start(out=lamin[:], in_=lam_d[:, :]), writes=["lamin"], dma_key="lamin")
    k.op("sp", lambda e: e.dma_start(out=gsub[:], in_=gsub_d[:, :]), writes=["gsub"], dma_key="gsub")
    k.op("pool", lambda e: e.memset(lams[:], 0.0), writes=["lams"])
    for j in range(2):
        k.op("dve", lambda e, j=j: e.tensor_tensor(out=lamj[:], in0=lamin[:, j * 64:j * 64 + 32], in1=lamin[:, j * 64 + 32:j * 64 + 64], op=ALU.mult),
             reads=["lamin"], writes=["lamj"])
        k.op("dve", lambda e, j=j: e.tensor_reduce(out=lams[:, j:j + 1], in_=lamj[:], axis=AX.X, op=ALU.add), reads=["lamj", "lams"], writes=["lams"])
    k.op("act", lambda e: e.activation(out=lams[:, 2:4], in_=lams[:, 0:2], func=AF.Exp), reads=["lams"], writes=["lams"])
    k.op("dve", lambda e: e.tensor_tensor(out=neglam[:], in0=lams[:, 3:4], in1=lams[:, 2:3], op=ALU.subtract), reads=["lams"], writes=["neglam"])
    k.op("dve", lambda e: e.tensor_scalar(out=neglam[:], in0=neglam[:], scalar1=-lam_init, scalar2=None, op0=ALU.add), reads=["neglam"], writes=["neglam"])
    k.op("dve", lambda e: e.tensor_scalar(out=gsub[:], in0=gsub[:], scalar1=(1.0 - lam_init), scalar2=None, op0=ALU.mult), reads=["gsub"], writes=["gsub"])

    arena = k.sb("arena", [128, 25090], BF16)
    kT = arena[:, 0:NKEYS]
    Vb = [arena[:, NKEYS:NKEYS + NKT * 65].rearrange("p (t c) -> p t c", c=65)] * 2
    catT = k.sb("catT", [128, 8, NT], BF16)
    if dbg.get("zero_cat"):
        k.op("pool", lambda e: e.memset(catT[:], 0.0), writes=["catT"])
    qTb = [k.sb("qTb0", [128, NT], BF16)] * 2
    PT = [k.sb("PT%d" % i, [128, 512], BF16) for i in range(4)]
    oa = [k.sb("oa%d" % i, [128, 512], F32) for i in range(2)]
    rz = [k.sb("rz%d" % i, [128, 512], F32) for i in range(2)]
    d1 = k.sb("d1", [64, 512], F32)
    d2 = k.sb("d2", [64, 512], F32)
    sqd = k.sb("sqd", [64, 512], F32)
    rsd = k.sb("rsd", [64, 512], F32)

    blocks = []
    if has_ctx:
        blocks.append((0, 256, 2))
    for b in range(dbg.get('nblk', 4)):
        blocks.append((CTX + b * 512, 512, dbg.get('nkt', NKT)))

    ocnt = [0]
    vcnt = [0]

    def attention(qbuf, qkey, po_list, kdim, vt, vkey, scale, epilogue):
        nm = len(po_list)
        for (tok0, nq, nkt) in blocks:
            obs = []
            for m in range(nm):
                obs.append(ocnt[0] % 4)
                ocnt[0] += 1
            U = nkt * nm
            LAG = 2
            for u in range(U + LAG):
                if u < U:
                    kt, m = divmod(u, nm)
                    po = po_list[m]
                    sb_i = u % 3
                    pt_i = u % 4
                    k.op("pe", lambda e, kt=kt, po=po, sb_i=sb_i, tok0=tok0, nq=nq: e.matmul(
                        Sb[sb_i][:, 0:nq], lhsT=kT[po:po + kdim, kt * 128:(kt + 1) * 128], rhs=qbuf[po:po + kdim, tok0:tok0 + nq],
                        start=True, stop=True, tile_position=(po, 0)),
                        reads=["kT", qkey], writes=["S%d" % sb_i])
                    k.op("act", lambda e, sb_i=sb_i, pt_i=pt_i, nq=nq: e.activation(out=PT[pt_i][:, 0:nq], in_=Sb[sb_i][:, 0:nq], func=AF.Exp, scale=scale),
                         reads=["S%d" % sb_i], writes=["PT%d" % pt_i])
                v = u - LAG
                if v >= 0:
                    kt, m = divmod(v, nm)
                    pt_i = v % 4
                    ob = obs[m]
                    k.op("pe", lambda e, kt=kt, pt_i=pt_i, ob=ob, nq=nq, nkt=nkt: e.matmul(
                        Ob[ob][0:65, 0:nq], lhsT=vt[:, kt, :], rhs=PT[pt_i][:, 0:nq], start=(kt == 0), stop=(kt == nkt - 1)),
                        reads=[vkey, "PT%d" % pt_i], writes=["O%d" % ob])
            epilogue(tok0, nq, obs)

    def norm_rows(i, ob, nq, extra_scale=None):
        k.op("dve", lambda e: e.tensor_copy(out=oa[i][0:65, 0:nq], in_=Ob[ob][0:65, 0:nq]), reads=["O%d" % ob], writes=["oa%d" % i])
        k.op("dve", lambda e: e.reciprocal(out=rz[i][64:65, 0:nq], in_=oa[i][64:65, 0:nq]), reads=["oa%d" % i], writes=["rz%d" % i])
        if extra_scale is not None:
            k.op("dve", lambda e: e.tensor_scalar(out=rz[i][64:65, 0:nq], in0=rz[i][64:65, 0:nq], scalar1=extra_scale[64:65, 0:1], scalar2=None, op0=ALU.mult),
                 reads=["rz%d" % i, "neglam"], writes=["rz%d" % i])
        k.op("pe", lambda e: e.matmul(Mb[0:64, 0:nq], lhsT=onesf[64:65, 0:64], rhs=rz[i][64:65, 0:nq], start=True, stop=True),
             reads=["onesf", "rz%d" % i], writes=["M"])

    for h in range(dbg.get('nd', 6)):
        qi = 0
        qkey = "qTb%d" % qi
        if h % 2 == 0:
            k.op("sp", lambda e, h=h: e.dma_start(out=kT, in_=kTd_d[h // 2, :, :]), writes=["kT"], dma_key="kT")
            k.op("sp", lambda e, h=h, qi=qi: e.dma_start(out=qTb[qi][:], in_=qT_d[h // 2, :, :]), writes=[qkey], dma_key=qkey)
        vi = 0
        vkey = "V%d" % vi
        k.op("sp", lambda e, h=h, vi=vi: e.dma_start(out=Vb[vi], in_=vaug_d[h, :, :].rearrange("p (t c) -> p t c", c=65)), writes=[vkey], dma_key=vkey)
        pb = 64 * (h % 2)
        chunk = 2 + h // 2

        def epi_diff(tok0, nq, obs, pb=pb, chunk=chunk):
            norm_rows(0, obs[0], nq)
            k.op("dve", lambda e: e.tensor_tensor(out=d1[:, 0:nq], in0=oa[0][0:64, 0:nq], in1=Mb[0:64, 0:nq], op=ALU.mult),
                 reads=["oa0", "M"], writes=["d1"])
            norm_rows(1, obs[1], nq, extra_scale=neglam)
            k.op("dve", lambda e: e.tensor_tensor(out=d2[:, 0:nq], in0=oa[1][0:64, 0:nq], in1=Mb[0:64, 0:nq], op=ALU.mult),
                 reads=["oa1", "M"], writes=["d2"])
            k.op("pool", lambda e: e.tensor_tensor(out=d1[:, 0:nq], in0=d1[:, 0:nq], in1=d2[:, 0:nq], op=ALU.add), reads=["d1", "d2"], writes=["d1"])
            k.op("pool", lambda e: e.tensor_tensor(out=sqd[:, 0:nq], in0=d1[:, 0:nq], in1=d1[:, 0:nq], op=ALU.mult), reads=["d1"], writes=["sqd"])
            k.op("pe", lambda e: e.matmul(Mb[0:64, 0:nq], lhsT=onesf[0:64, 0:64], rhs=sqd[:, 0:nq], start=True, stop=True),
                 reads=["onesf", "sqd"], writes=["M"])
            k.op("act", lambda e: e.activation(out=rsd[:, 0:nq], in_=Mb[0:64, 0:nq], func=AF.Sqrt, bias=epss[0:64, :], scale=1.0 / 64),
                 reads=["M", "eps"], writes=["rsd"])
            k.op("dve", lambda e: e.reciprocal(out=rsd[:, 0:nq], in_=rsd[:, 0:nq]), reads=["rsd"], writes=["rsd"])
            k.op("dve", lambda e: e.scalar_tensor_tensor(out=catT[pb:pb + 64, chunk, tok0:tok0 + nq], in0=d1[:, 0:nq], scalar=gsub[0:64, 0:1],
                                                         in1=rsd[:, 0:nq], op0=ALU.mult, op1=ALU.mult),
                 reads=["d1", "rsd", "gsub"], writes=["catT"])

        attention(qTb[qi], qkey, [pb, pb + 32], 32, Vb[vi], vkey, DIFF_SCALE, epi_diff)

    for h in range(dbg.get('ng', 6)):
        kv = h // 3
        qi = 0
        qkey = "qTb%d" % qi
        if h % 2 == 0:
            k.op("sp", lambda e, h=h, qi=qi: e.dma_start(out=qTb[qi][:], in_=qT_d[3 + h // 2, :, :]), writes=[qkey], dma_key=qkey)
        if h % 3 == 0:
            k.op("sp", lambda e, kv=kv: e.dma_start(out=kT, in_=kTg_d[kv, :, :]), writes=["kT"], dma_key="kT")
            vi = 0
            vkey = "V%d" % vi
            k.op("sp", lambda e, kv=kv, vi=vi: e.dma_start(out=Vb[vi], in_=vaug_d[6 + kv, :, :].rearrange("p (t c) -> p t c", c=65)),
                 writes=[vkey], dma_key=vkey)
        pb = 64 * (h % 2)
        chunk = 5 + h // 2

        def epi_gqa(tok0, nq, obs, pb=pb, chunk=chunk):
            norm_rows(0, obs[0], nq)
            k.op("dve", lambda e: e.tensor_tensor(out=catT[pb:pb + 64, chunk, tok0:tok0 + nq], in0=oa[0][0:64, 0:nq], in1=Mb[0:64, 0:nq], op=ALU.mult),
                 reads=["oa0", "M"], writes=["catT"])

        attention(qTb[qi], qkey, [pb], 64, Vb[vi], vkey, GQA_SCALE, epi_gqa)

    wfs = k.sb("wfs", [128, 2, 64], F32)
    dftc = k.sb("dftc", [128, 128], F32)
    dfts = k.sb("dfts", [128, 128], F32)
    MP = k.sb("MP", [128, 2, 128], BF16)
    MQ = k.sb("MQ", [128, 2, 128], BF16)
    k.op("sp", lambda e: e.dma_start(out=wfs[:], in_=wf_d.rearrange("c p e -> p c e")), writes=["wfs"], dma_key="wfs")
    k.op("sp", lambda e: e.dma_start(out=dftc[:], in_=dftc_d[:, :]), writes=["dftc"], dma_key="dftc")
    k.op("sp", lambda e: e.dma_start(out=dfts[:], in_=dfts_d[:, :]), writes=["dfts"], dma_key="dfts")
    k.op("pool", lambda e: e.memset(MP[:], 0.0), writes=["MP"])
    k.op("pool", lambda e: e.memset(MQ[:], 0.0), writes=["MQ"])
    for cc in range(2):
        for (tabm, dst, sgn, key) in ((dftc, MP, 1.0, "MP"), (dfts, MQ, -1.0, "MQ")):
            k.op("pe", lambda e, cc=cc, tabm=tabm: e.matmul(Mb[:, 0:64], lhsT=tabm[:], rhs=wfs[:, cc, :], start=True, stop=True),
                 reads=["wfs", "dftc", "dfts"], writes=["M"])
            for hh in range(2):
                k.op("dve", lambda e, cc=cc, dst=dst, sgn=sgn, hh=hh: e.tensor_scalar(
                    out=dst[hh * 64:(hh + 1) * 64, cc, hh * 64:(hh + 1) * 64], in0=Mb[hh * 64:(hh + 1) * 64, 0:64], scalar1=sgn, scalar2=None, op0=ALU.mult),
                    reads=["M"], writes=[key])
    tC = [k.sb("tC%d" % i, [128, 4, 512], BF16) for i in range(2)]
    tS = [k.sb("tS%d" % i, [128, 4, 512], BF16) for i in range(2)]
    fg = [k.sb("fg%d" % i, [128, 4, 256], BF16) for i in range(2)]
    Pb_ = [k.sb("Pb%d" % i, [128, 512], BF16) for i in range(2)]
    Qb_ = [k.sb("Qb%d" % i, [128, 512], BF16) for i in range(2)]
    accP = [Sb[0], Sb[1]]
    accQ = [Sb[2], Ob[2]]
    accPk = ["S0", "S1"]
    accQk = ["S2", "O2"]
    gcnt = [0]

    def dft_block(tok0, nq, ngroups, gl, loadfn, scale):
        ntl = ngroups * gl
        for g in range(ngroups):
            s = gcnt[0] % 2
            gcnt[0] += 1
            loadfn(g, s)
            for j in range(gl):
                lt = g * gl + j
                for cc in range(2):
                    k.op("pe", lambda e, s=s, j=j, cc=cc, lt=lt: e.matmul(accP[cc][:, 0:nq], lhsT=fg[s][:, j, cc * 128:(cc + 1) * 128], rhs=tC[s][:, j, 0:nq],
                                                                         start=(lt == 0), stop=(lt == ntl - 1)),
                         reads=["fg%d" % s, "tC%d" % s], writes=[accPk[cc]])
                    k.op("pe", lambda e, s=s, j=j, cc=cc, lt=lt: e.matmul(accQ[cc][:, 0:nq], lhsT=fg[s][:, j, cc * 128:(cc + 1) * 128], rhs=tS[s][:, j, 0:nq],
                                                                         start=(lt == 0), stop=(lt == ntl - 1)),
                         reads=["fg%d" % s, "tS%d" % s], writes=[accQk[cc]])
        for cc in range(2):
            k.op("act", lambda e, cc=cc: e.activation(out=Pb_[cc][:, 0:nq], in_=accP[cc][:, 0:nq], func=AF.Copy, scale=scale), reads=[accPk[cc]], writes=["Pb%d" % cc])
            k.op("act", lambda e, cc=cc: e.activation(out=Qb_[cc][:, 0:nq], in_=accQ[cc][:, 0:nq], func=AF.Copy, scale=scale), reads=[accQk[cc]], writes=["Qb%d" % cc])
        for cc in range(2):
            k.op("pe", lambda e, cc=cc: e.matmul(Mb[:, 0:nq], lhsT=MP[:, cc, :], rhs=Pb_[cc][:, 0:nq], start=True, stop=False), reads=["MP", "Pb%d" % cc], writes=["M"])
            k.op("pe", lambda e, cc=cc: e.matmul(Mb[:, 0:nq], lhsT=MQ[:, cc, :], rhs=Qb_[cc][:, 0:nq], start=False, stop=True), reads=["MQ", "Qb%d" % cc], writes=["M"])
            k.op("dve", lambda e, cc=cc: e.tensor_copy(out=catT[:, cc, tok0:tok0 + nq], in_=Mb[:, 0:nq]), reads=["M"], writes=["catT"])

    if has_ctx and dbg.get('dftctx', True):
        def load_ctx(g, s):
            k.op("sp", lambda e: e.dma_start(out=tC[s][:, 0:2, 0:256], in_=tabCc_d.rearrange("p (j i) -> p j i", j=2)), writes=["tC%d" % s], dma_key="tC%d" % s)
            k.op("sp", lambda e: e.dma_start(out=tS[s][:, 0:2, 0:256], in_=tabSc_d.rearrange("p (j i) -> p j i", j=2)), writes=["tS%d" % s], dma_key="tS%d" % s)
            k.op("sp", lambda e: e.dma_start(out=fg[s][:, 0:2, :], in_=fctx_d.rearrange("p (j i) -> p j i", j=2)), writes=["fg%d" % s], dma_key="fg%d" % s)
        dft_block(0, 256, 1, 2, load_ctx, 1.0 / math.sqrt(CTX * 64.0))
    onesb = k.sb("onesb", [1, 128], BF16)
    k.op("pool", lambda e: e.memset(onesb[:], 1.0), writes=["onesb"])
    dbase = k.sb("dbase", [128, 4, 1024], BF16)
    k.op("sp", lambda e: e.dma_start(out=dbase[:], in_=dbase_d.rearrange("p (b i) -> p b i", b=4)), writes=["dbase"], dma_key="dbase")
    abrow = [k.sb("abrow%d" % i, [1, 4 * 1024], BF16) for i in range(2)]
    mt = [k.sb("mt%d" % i, [128, 512], BF16) for i in range(4)]
    psAb = Ob[0]
    psBb = Ob[1]
    for kb in range(dbg.get('ndft', 4)):
        def load_lat(g, s, kb=kb):
            k.op("sp", lambda e: e.dma_start(out=fg[s][:], in_=fall_d[g, :, :].rearrange("p (j i) -> p j i", j=4)), writes=["fg%d" % s], dma_key="fg%d" % s)
            k.op("sp", lambda e: e.dma_start(out=abrow[s][:], in_=ab_d[g:g + 1, :]), writes=["abrow%d" % s], dma_key="abrow%d" % s)
            cb = dbase[:, kb, 0:512]
            sbb = dbase[:, kb, 512:1024]
            for j in range(4):
                k.op("pe", lambda e, j=j: e.matmul(psAb, lhsT=onesb[0:1, 0:128], rhs=abrow[s][0:1, j * 1024:j * 1024 + 512], start=True, stop=True),
                     reads=["onesb", "abrow%d" % s], writes=["O0"])
                k.op("pe", lambda e, j=j: e.matmul(psBb, lhsT=onesb[0:1, 0:128], rhs=abrow[s][0:1, j * 1024 + 512:(j + 1) * 1024], start=True, stop=True),
                     reads=["onesb", "abrow%d" % s], writes=["O1"])
                k.op("dve", lambda e: e.tensor_tensor(out=mt[0][:], in0=psAb, in1=cb, op=ALU.mult), reads=["O0", "dbase"], writes=["mt0"])
                k.op("dve", lambda e: e.tensor_tensor(out=mt[1][:], in0=psBb, in1=sbb, op=ALU.mult), reads=["O1", "dbase"], writes=["mt1"])
                k.op("dve", lambda e: e.tensor_tensor(out=mt[2][:], in0=psAb, in1=sbb, op=ALU.mult), reads=["O0", "dbase"], writes=["mt2"])
                k.op("dve", lambda e: e.tensor_tensor(out=mt[3][:], in0=psBb, in1=cb, op=ALU.mult), reads=["O1", "dbase"], writes=["mt3"])
                k.op("pool", lambda e, j=j: e.tensor_tensor(out=tC[s][:, j, :], in0=mt[0][:], in1=mt[1][:], op=ALU.subtract), reads=["mt0", "mt1"], writes=["tC%d" % s])
                k.op("pool", lambda e, j=j: e.tensor_tensor(out=tS[s][:, j, :], in0=mt[2][:], in1=mt[3][:], op=ALU.add), reads=["mt2", "mt3"], writes=["tS%d" % s])
        dft_block(CTX + kb * 512, 512, 32, 4, load_lat, 1.0 / math.sqrt(SEQ * 64.0))

    fence = k.sb("fence", [128, 1], F32)
    B3K = ["woutb", "xt0", "xt1", "tmp", "h2Tf"]
    k.op("dve", lambda e: e.memset(fence[:], 0.0), writes=["kT", "V0", "V1"] + B3K)
    woutb = arena[:, 0:8192].rearrange("p (c n) -> p c n", c=8)
    xt = [arena[:, 8192:10240].bitcast(F32), arena[:, 10240:12288].bitcast(F32)]
    tmp_t = k.sb("tmp_t", [128, D], F32)
    tmp = tmp_t[:]
    for kk in range(8):
        k.op("pool", lambda e, kk=kk: e.dma_start(out=woutb[:, kk, :], in_=wout_d[kk * 128:(kk + 1) * 128, :]), reads=["woutb"], writes=["woutb_l"], dma_key="woutb")
    ps_o = psA
    ps_tr = psC
    ps_lg = psB[:, 0:32]
    ps_gt = psB[0:32, 512:640]
    tiles = list(range(NTILES)) if has_ctx else list(range(2, NTILES))
    tiles = tiles[:dbg.get('nb3', len(tiles))]
    if not has_ctx:
        k.op("sp", lambda e: e.dma_start(out=x1_o[0:CTX, :], in_=xin[0:CTX, :]), dma_key="ctxcopy")
    for t in tiles:
        s = t % 2
        mi = 1 if t < 2 else 0
        xk = "xt%d" % s
        k.op("sp", lambda e, s=s, t=t: e.dma_start(out=xt[s], in_=xin[t * 128:(t + 1) * 128, :]), writes=[xk], dma_key=xk)
        for cb in range(2):
            for ch in range(8):
                k.op("pe", lambda e, cb=cb, ch=ch, t=t: e.matmul(ps_o[:, cb * 512:(cb + 1) * 512], lhsT=catT[:, ch, t * 128:(t + 1) * 128],
                                                                 rhs=woutb[:, ch, cb * 512:(cb + 1) * 512], start=(ch == 0), stop=(ch == 7)),
                     reads=["catT", "woutb_l"], writes=["S0", "S1"])
        k.op("dve", lambda e, mi=mi: e.tensor_tensor(out=tmp, in0=ps_o[:, :], in1=mods[mi][:, 0:1024], op=ALU.mult), reads=["S0", "S1", "mod"], writes=["tmp"])
        k.op("pool", lambda e, s=s: e.tensor_tensor(out=xt[s], in0=tmp, in1=xt[s], op=ALU.add), reads=["tmp", xk], writes=[xk])
        k.op("sp", lambda e, s=s, t=t: e.dma_start(out=x1_o[t * 128:(t + 1) * 128, :], in_=xt[s]), reads=[xk], dma_key=xk + "o")
    return k


NTH = NT // 2
HBLK = [(0, 512), (512, 512), (1024, 128)]


def build_C(final=True, dbg=None):
    dbg = dbg or {}
    final = True
    nocc = dbg.get("nocc", False)
    full = dbg.get("full", False)
    nexp = dbg.get("nexp", NEXP)
    k = KB()
    nc = k.nc
    x1_d = k.din("x1", [NT, D], F32)
    csil_d = k.din("csil", [128, 16], F32)
    wmod_d = k.din("wmod", [D, 3072], F32)
    bmod_d = k.din("bmod", [128, 3072], F32)
    g2_d = k.din("g2", [128, D], F32)
    wr_d = k.din("wr", [128, 8 * 32], F32)
    br_d = k.din("br", [128, 32], F32)
    wsh_d = k.din("wsh", [768 if full else 96, 128, 1024], F32)
    bgu_d = k.din("bgu", [128, 512], F32)
    bd_d = k.din("bd", [32, D], F32)
    gfin_d = k.din("gfin", [128, D], F32)
    xo = k.dout("xo", [NT, D], F32)
    xn = k.dout("xn", [NT, D], F32)
    if not full:
        Gws = [nc.dram_tensor("Gw%d" % i, [24, 8 * 128, 1024], F32).ap() for i in range(4)]

    def GwU(u):
        return Gws[u // 24][u % 24]

    def wsrc(ex_i, v):
        if full:
            return wsh_d[ex_i * 24 + v, :, :]
        r, el = divmod(ex_i, 4)
        return GwU(el * 24 + v)[r * 128:(r + 1) * 128, :]

    bnc = [nc.dram_tensor("bnc%d" % i, [128, 1024], F32).ap() for i in range(2)]

    identf, identb = make_identities(k)
    psT = k.ps("psT", [128, 2048], BF16)
    psM = k.ps("psM", [128, 1024], F32)
    psG = k.ps("psG", [128, 2048], F32)

    for u in range(0 if full else 96):
        b = u % 2
        if nocc:
            k.op("pool", lambda e, u=u: e.dma_start(out=GwU(u)[0:128, :], in_=wsh_d[u, :, :]), writes=["Gw"], dma_key="gwd")
        else:
            k.op("pool", lambda e, u=u, b=b: e.dma_start(out=bnc[b][:, :], in_=wsh_d[u, :, :]), writes=["bnc%d" % b], dma_key="bnc%d" % b)
            k.op("pool", lambda e, u=u, b=b: e.collective_compute("AllGather", ALU.bypass, replica_groups=[list(range(NCORES))],
                                                                 ins=[bnc[b][:, :].opt()], outs=[GwU(u)[:, :].opt()]),
                 reads=["bnc%d" % b], writes=["Gw"], dma_key="cc", inc=1)

    mod_lat = k.sb("mod_lat", [128, 3072], F32)
    mod_ctx = k.sb("mod_ctx", [128, 3072], F32)
    mods = (mod_lat, mod_ctx)
    emit_mod(k, csil_d, wmod_d, bmod_d, [0, 1, 2, 3, 4, 5], mod_lat, mod_ctx, psM, "psM0", nbuf=1)
    g2 = k.sb("g2", [128, D], F32)
    k.op("sp", lambda e: e.dma_start(out=g2[:], in_=g2_d[:, :]), writes=["g2"], dma_key="g2")
    for i in range(2):
        k.op("dve", lambda e, i=i: e.scalar_tensor_tensor(out=mods[i][:, 1024:2048], in0=mods[i][:, 1024:2048], scalar=1.0, in1=g2[:],
                                                           op0=ALU.add, op1=ALU.mult), reads=["mod", "g2"], writes=["mod"])
    gfin = k.sb("gfin", [128, D], F32)
    k.op("sp", lambda e: e.dma_start(out=gfin[:], in_=gfin_d[:, :]), writes=["gfin"], dma_key="gfin")
    epsn = k.sb("epsn", [128, 1], F32)
    k.op("pool", lambda e: e.memset(epsn[:], NORM_EPS), writes=["eps"])
    selb = k.sb("selb", [32, 32, 128], BF16)
    k.op("pool", lambda e: e.memset(selb[:], 1.0), writes=["selb"])
    k.op("pool", lambda e: e.affine_select(out=selb[:], in_=selb[:], pattern=[[-1, 32], [0, 128]], compare_op=ALU.is_equal,
                                           fill=0.0, base=0, channel_multiplier=1), reads=["selb"], writes=["selb"])
    wr = k.sb("wr", [128, 8, 32], F32)
    wr_hi = k.sb("wr_hi", [128, 8, 32], BF16)
    wr_lo = k.sb("wr_lo", [128, 8, 32], BF16)
    brt = k.sb("brt", [128, 32], F32)
    k.op("sp", lambda e: e.dma_start(out=wr[:], in_=wr_d.rearrange("p (c n) -> p c n", c=8)), writes=["wr"], dma_key="wr")
    k.op("sp", lambda e: e.dma_start(out=brt[:], in_=br_d[:, :]), writes=["brt"], dma_key="brt")
    k.op("dve", lambda e: e.tensor_copy(out=wr_hi[:], in_=wr[:]), reads=["wr"], writes=["wr_hi"])
    k.op("dve", lambda e: e.tensor_tensor(out=wr_lo[:], in0=wr[:], in1=wr_hi[:], op=ALU.subtract), reads=["wr", "wr_hi"], writes=["wr_lo"])
    bd = k.sb("bd", [32, D], F32)
    bd_hi = k.sb("bd_hi", [32, D], BF16)
    bd_lo = k.sb("bd_lo", [32, D], BF16)
    k.op("sp", lambda e: e.dma_start(out=bd[:], in_=bd_d[:, :]), writes=["bd"], dma_key="bd")
    k.op("dve", lambda e: e.tensor_copy(out=bd_hi[:], in_=bd[:]), reads=["bd"], writes=["bd_hi"])
    k.op("dve", lambda e: e.tensor_tensor(out=bd_lo[:], in0=bd[:], in1=bd_hi[:], op=ALU.subtract), reads=["bd", "bd_hi"], writes=["bd_lo"])
    bgu = k.sb("bgu", [128, 512], F32)
    bgu1 = k.sb("bgu1", [128, 512], F32)
    k.op("sp", lambda e: e.dma_start(out=bgu[:], in_=bgu_d[:, :]), writes=["bgu"], dma_key="bgu")
    k.op("dve", lambda e: e.tensor_scalar(out=bgu1[:], in0=bgu[:], scalar1=1.0, scalar2=None, op0=ALU.add), reads=["bgu"], writes=["bgu1"])

    h2T = k.sb("h2T", [128, 8, NTH], BF16)
    Yacc = k.sb("Yacc", [128, 8, NTH], F32)
    actT = k.sb("actT", [128, 8, NTH], BF16)
    gb = k.sb("gb", [128, NTH], F32)
    GT_hi = k.sb("GT_hi", [32, NTH], BF16)
    GT_lo = k.sb("GT_lo", [32, NTH], BF16)
    xt = [k.sb("xt%d" % i, [128, D], F32) for i in range(2)]
    tmp = k.sb("tmp", [128, D], F32)
    h2f = k.sb("h2f", [128, D], F32)
    hi = k.sb("hi", [128, D], BF16)
    lo = k.sb("lo", [128, D], BF16)
    loT = k.sb("loT", [128, D], BF16)
    ss = k.sb("ss", [128, 2 * NTILES], F32)
    rstd = k.sb("rstd", [128, 2 * NTILES], F32)
    k.op("pool", lambda e: e.memset(ss[:], 0.0), writes=["ss"])
    lg = k.sb("lg", [128, 32], F32)
    m8 = k.sb("m8", [128, 8], F32)
    msk = k.sb("msk", [128, 32], F32)
    negm = k.sb("negm", [128, 1], F32)
    ex = k.sb("ex", [128, 32], F32)
    em = k.sb("em", [128, 32], F32)
    sm = k.sb("sm", [128, NTILES], F32)
    gts = k.sb("gts", [128, 32], F32)
    g_hi = k.sb("g_hi", [128, 32], BF16)
    g_lo = k.sb("g_lo", [128, 32], BF16)
    wg = [k.sb("wg%d" % i, [128, 8, 256], BF16) for i in range(3)]
    wd = [k.sb("wd%d" % i, [128, 8, 128], BF16) for i in range(3)]
    gs = k.sb("gs", [128, 512], F32)
    sg = k.sb("sg", [128, 512], F32)
    us = k.sb("us", [128, 512], F32)
    tt = k.sb("tt", [128, 512], F32)
    ps_lg = psM[:, 0:32]
    wgc = [0]
    wdc = [0]
    gsl = [0]

    for hf in range(2):
        for lt in range(9):
            t = hf * 9 + lt
            s = t % 2
            mi = 1 if t < 2 else 0
            xk = "xt%d" % s
            k.op("sp", lambda e, s=s, t=t: e.dma_start(out=xt[s][:], in_=x1_d[t * 128:(t + 1) * 128, :]), writes=[xk], dma_key=xk)
            k.op("act", lambda e, s=s, t=t: e.activation(out=tmp[:], in_=xt[s][:], func=AF.Square, accum_out=ss[:, t:t + 1]), reads=[xk, "ss"], writes=["tmp", "ss"])
            k.op("act", lambda e, t=t: e.activation(out=rstd[:, t:t + 1], in_=ss[:, t:t + 1], func=AF.Sqrt, bias=epsn[:], scale=1.0 / D), reads=["ss", "eps"], writes=["rstd"])
            k.op("dve", lambda e, t=t: e.reciprocal(out=rstd[:, t:t + 1], in_=rstd[:, t:t + 1]), reads=["rstd"], writes=["rstd"])
            k.op("dve", lambda e, s=s, t=t, mi=mi: e.scalar_tensor_tensor(out=tmp[:], in0=xt[s][:], scalar=rstd[:, t:t + 1], in1=mods[mi][:, 1024:2048],
                                                                          op0=ALU.mult, op1=ALU.mult), reads=[xk, "rstd", "mod"], writes=["tmp"])
            k.op("pool", lambda e, mi=mi: e.tensor_tensor(out=h2f[:], in0=tmp[:], in1=mods[mi][:, 0:1024], op=ALU.add), reads=["tmp", "mod"], writes=["h2f"])
            k.op("dve", lambda e: e.tensor_copy(out=hi[:], in_=h2f[:]), reads=["h2f"], writes=["hi"])
            k.op("dve", lambda e: e.tensor_tensor(out=lo[:], in0=h2f[:], in1=hi[:], op=ALU.subtract), reads=["h2f", "hi"], writes=["lo"])
            for j in range(8):
                k.op("pe", lambda e, j=j: e.transpose(out=psT[:, j * 128:(j + 1) * 128], in_=hi[:, j * 128:(j + 1) * 128], identity=identb[:]),
                     reads=["hi", "identb"], writes=["psT"])
            for j in range(8):
                k.op("pe", lambda e, j=j: e.transpose(out=psT[:, 1024 + j * 128:1024 + (j + 1) * 128], in_=lo[:, j * 128:(j + 1) * 128], identity=identb[:]),
                     reads=["lo", "identb"], writes=["psT"])
            k.op("act", lambda e, lt=lt: e.activation(out=h2T[:, :, lt * 128:(lt + 1) * 128], in_=psT[:, 0:1024].rearrange("p (j t) -> p j t", j=8), func=AF.Copy),
                 reads=["psT"], writes=["h2T"])
            k.op("dve", lambda e: e.tensor_copy(out=loT[:], in_=psT[:, 1024:2048]), reads=["psT"], writes=["loT"])
            for kk in range(8):
                k.op("pe", lambda e, kk=kk, lt=lt: e.matmul(ps_lg, lhsT=h2T[:, kk, lt * 128:(lt + 1) * 128], rhs=wr_hi[:, kk, :], start=(kk == 0), stop=False),
                     reads=["h2T", "wr_hi"], writes=["psM0"])
                k.op("pe", lambda e, kk=kk, lt=lt: e.matmul(ps_lg, lhsT=h2T[:, kk, lt * 128:(lt + 1) * 128], rhs=wr_lo[:, kk, :], start=False, stop=False),
                     reads=["h2T", "wr_lo"], writes=["psM0"])
                k.op("pe", lambda e, kk=kk: e.matmul(ps_lg, lhsT=loT[:, kk * 128:(kk + 1) * 128], rhs=wr_hi[:, kk, :], start=False, stop=(kk == 7)),
                     reads=["loT", "wr_hi"], writes=["psM0"])
            k.op("dve", lambda e: e.tensor_tensor(out=lg[:], in0=ps_lg, in1=brt[:], op=ALU.add), reads=["psM0", "brt"], writes=["lg"])
            k.op("dve", lambda e: e.max(out=m8[:], in_=lg[:]), reads=["lg"], writes=["m8"])
            k.op("dve", lambda e: e.tensor_scalar(out=msk[:], in0=lg[:], scalar1=m8[:, 3:4], scalar2=None, op0=ALU.is_ge), reads=["lg", "m8"], writes=["msk"])
            k.op("dve", lambda e: e.tensor_scalar(out=negm[:], in0=m8[:, 0:1], scalar1=-1.0, scalar2=None, op0=ALU.mult), reads=["m8"], writes=["negm"])
            k.op("act", lambda e: e.activation(out=ex[:], in_=lg[:], func=AF.Exp, bias=negm[:], scale=1.0), reads=["lg", "negm"], writes=["ex"])
            k.op("dve", lambda e: e.tensor_tensor(out=em[:], in0=ex[:], in1=msk[:], op=ALU.mult), reads=["ex", "msk"], writes=["em"])
            k.op("dve", lambda e, t=t: e.tensor_reduce(out=sm[:, t:t + 1], in_=em[:], axis=AX.X, op=ALU.add), reads=["em"], writes=["sm"])
            k.op("dve", lambda e, t=t: e.reciprocal(out=sm[:, t:t + 1], in_=sm[:, t:t + 1]), reads=["sm"], writes=["sm"])
            k.op("dve", lambda e, t=t: e.tensor_scalar(out=gts[:], in0=em[:], scalar1=sm[:, t:t + 1], scalar2=None, op0=ALU.mult), reads=["em", "sm"], writes=["gts"])
            k.op("dve", lambda e: e.tensor_copy(out=g_hi[:], in_=gts[:]), reads=["gts"], writes=["g_hi"])
            k.op("dve", lambda e: e.tensor_tensor(out=g_lo[:], in0=gts[:], in1=g_hi[:], op=ALU.subtract), reads=["gts", "g_hi"], writes=["g_lo"])
            k.op("pe", lambda e: e.transpose(out=psT[0:32, 0:128], in_=g_hi[:], identity=identb[:]), reads=["g_hi", "identb"], writes=["psT"])
            k.op("pe", lambda e: e.transpose(out=psT[0:32, 128:256], in_=g_lo[:], identity=identb[:]), reads=["g_lo", "identb"], writes=["psT"])
            k.op("dve", lambda e, lt=lt: e.tensor_copy(out=GT_hi[:, lt * 128:(lt + 1) * 128], in_=psT[0:32, 0:128]), reads=["psT"], writes=["GT_hi"])
            k.op("dve", lambda e, lt=lt: e.tensor_copy(out=GT_lo[:, lt * 128:(lt + 1) * 128], in_=psT[0:32, 128:256]), reads=["psT"], writes=["GT_lo"])
        for (b0, nb) in HBLK:
            for dc in range(8):
                k.op("pe", lambda e, dc=dc, b0=b0, nb=nb: e.matmul(psM[:, 0:nb], lhsT=bd_hi[:, dc * 128:(dc + 1) * 128], rhs=GT_hi[:, b0:b0 + nb], start=True, stop=False),
                     reads=["bd_hi", "GT_hi"], writes=["psM0"])
                k.op("pe", lambda e, dc=dc, b0=b0, nb=nb: e.matmul(psM[:, 0:nb], lhsT=bd_hi[:, dc * 128:(dc + 1) * 128], rhs=GT_lo[:, b0:b0 + nb], start=False, stop=False),
                     reads=["bd_hi", "GT_lo"], writes=["psM0"])
                k.op("pe", lambda e, dc=dc, b0=b0, nb=nb: e.matmul(psM[:, 0:nb], lhsT=bd_lo[:, dc * 128:(dc + 1) * 128], rhs=GT_hi[:, b0:b0 + nb], start=False, stop=True),
                     reads=["bd_lo", "GT_hi"], writes=["psM0"])
                k.op("act", lambda e, dc=dc, b0=b0, nb=nb: e.activation(out=Yacc[:, dc, b0:b0 + nb], in_=psM[:, 0:nb], func=AF.Copy), reads=["psM0"], writes=["Yacc"])
        for ex_i in range(nexp):
            r, el = divmod(ex_i, 4)
            for (b0, nb) in HBLK:
                k.op("pe", lambda e, ex_i=ex_i, b0=b0, nb=nb: e.matmul(psM[:, 0:nb], lhsT=selb[:, ex_i, :], rhs=GT_hi[:, b0:b0 + nb], start=True, stop=False),
                     reads=["selb", "GT_hi"], writes=["psM0"])
                k.op("pe", lambda e, ex_i=ex_i, b0=b0, nb=nb: e.matmul(psM[:, 0:nb], lhsT=selb[:, ex_i, :], rhs=GT_lo[:, b0:b0 + nb], start=False, stop=True),
                     reads=["selb", "GT_lo"], writes=["psM0"])
                k.op("act", lambda e, b0=b0, nb=nb: e.activation(out=gb[:, b0:b0 + nb], in_=psM[:, 0:nb], func=AF.Copy), reads=["psM0"], writes=["gb"])
            for j in range(8):
                s = wgc[0] % 3
                wgc[0] += 1
                for h2 in range(2):
                    u = el * 24 + j * 2 + h2
                    k.op("pool", lambda e, s=s, h2=h2, ex_i=ex_i, j=j: e.dma_start(out=wg[s][:, h2 * 4:(h2 + 1) * 4, :],
                                                                            in_=wsrc(ex_i, j * 2 + h2).rearrange("p (k c) -> p k c", k=4)),
                         reads=["Gw"], writes=["wg%d" % s], dma_key="wg%d" % s)
                for (b0, nb) in HBLK:
                    q = gsl[0] % 2
                    gsl[0] += 1
                    pg = psG[:, q * 1024:q * 1024 + nb]
                    pl = psG[:, q * 1024 + 512:q * 1024 + 512 + nb]
                    for kk in range(8):
                        k.op("pe", lambda e, s=s, kk=kk, pg=pg, b0=b0, nb=nb: e.matmul(pg, lhsT=wg[s][:, kk, 0:128], rhs=h2T[:, kk, b0:b0 + nb], start=(kk == 0), stop=(kk == 7)),
                             reads=["wg%d" % s, "h2T"], writes=["psG%dg" % q])
                    for kk in range(8):
                        k.op("pe", lambda e, s=s, kk=kk, pl=pl, b0=b0, nb=nb: e.matmul(pl, lhsT=wg[s][:, kk, 128:256], rhs=h2T[:, kk, b0:b0 + nb], start=(kk == 0), stop=(kk == 7)),
                             reads=["wg%d" % s, "h2T"], writes=["psG%dl" % q])
                    cg = ex_i * 16 + j
                    cl = ex_i * 16 + 8 + j
                    k.op("dve", lambda e, pg=pg, nb=nb, cg=cg: e.tensor_scalar(out=gs[:, 0:nb], in0=pg, scalar1=bgu[:, cg:cg + 1], scalar2=7.0, op0=ALU.add, op1=ALU.min),
                         reads=["psG%dg" % q, "bgu"], writes=["gs"])
                    k.op("act", lambda e, nb=nb: e.activation(out=sg[:, 0:nb], in_=gs[:, 0:nb], func=AF.Sigmoid, scale=1.702), reads=["gs"], writes=["sg"])
                    k.op("dve", lambda e, pl=pl, nb=nb, cl=cl: e.tensor_scalar(out=us[:, 0:nb], in0=pl, scalar1=bgu1[:, cl:cl + 1], scalar2=8.0, op0=ALU.add, op1=ALU.min),
                         reads=["psG%dl" % q, "bgu1"], writes=["us"])
                    k.op("dve", lambda e, nb=nb: e.tensor_tensor(out=tt[:, 0:nb], in0=gs[:, 0:nb], in1=sg[:, 0:nb], op=ALU.mult), reads=["gs", "sg"], writes=["tt"])
                    k.op("dve", lambda e, nb=nb, b0=b0: e.tensor_tensor(out=tt[:, 0:nb], in0=tt[:, 0:nb], in1=gb[:, b0:b0 + nb], op=ALU.mult), reads=["tt", "gb"], writes=["tt"])
                    k.op("dve", lambda e, nb=nb, b0=b0, j=j: e.scalar_tensor_tensor(out=actT[:, j, b0:b0 + nb], in0=us[:, 0:nb], scalar=-6.0, in1=tt[:, 0:nb],
                                                                                    op0=ALU.max, op1=ALU.mult), reads=["us", "tt"], writes=["actT"])
            for dc in range(8):
                s = wdc[0] % 3
                wdc[0] += 1
                u = el * 24 + 16 + dc
                k.op("pool", lambda e, s=s, ex_i=ex_i, dc=dc: e.dma_start(out=wd[s][:], in_=wsrc(ex_i, 16 + dc).rearrange("p (k c) -> p k c", k=8)),
                     reads=["Gw"], writes=["wd%d" % s], dma_key="wd%d" % s)
                for (b0, nb) in HBLK:
                    for jj in range(8):
                        k.op("pe", lambda e, s=s, jj=jj, b0=b0, nb=nb: e.matmul(psM[:, 512:512 + nb], lhsT=wd[s][:, jj, :], rhs=actT[:, jj, b0:b0 + nb], start=(jj == 0), stop=(jj == 7)),
                             reads=["wd%d" % s, "actT"], writes=["psY"])
                    k.op("dve", lambda e, dc=dc, b0=b0, nb=nb: e.tensor_tensor(out=Yacc[:, dc, b0:b0 + nb], in0=psM[:, 512:512 + nb], in1=Yacc[:, dc, b0:b0 + nb], op=ALU.add),
                         reads=["psY", "Yacc"], writes=["Yacc"])
        for lt in range(9):
            t = hf * 9 + lt
            s = t % 2
            mi = 1 if t < 2 else 0
            xk = "xt%d" % s
            k.op("sp", lambda e, s=s, t=t: e.dma_start(out=xt[s][:], in_=x1_d[t * 128:(t + 1) * 128, :]), writes=[xk], dma_key=xk)
            hi3 = hi[:].rearrange("p (j t) -> p j t", j=8)
            lo3 = lo[:].rearrange("p (j t) -> p j t", j=8)
            k.op("dve", lambda e, lt=lt, hi3=hi3: e.tensor_copy(out=hi3, in_=Yacc[:, :, lt * 128:(lt + 1) * 128]), reads=["Yacc"], writes=["hi"])
            k.op("pool", lambda e, lt=lt, hi3=hi3, lo3=lo3: e.tensor_tensor(out=lo3, in0=Yacc[:, :, lt * 128:(lt + 1) * 128], in1=hi3, op=ALU.subtract), reads=["Yacc", "hi"], writes=["lo"])
            for j in range(8):
                k.op("pe", lambda e, j=j: e.transpose(out=psT[:, j * 128:(j + 1) * 128], in_=hi[:, j * 128:(j + 1) * 128], identity=identb[:]), reads=["hi", "identb"], writes=["psT"])
            for j in range(8):
                k.op("pe", lambda e, j=j: e.transpose(out=psT[:, 1024 + j * 128:1024 + (j + 1) * 128], in_=lo[:, j * 128:(j + 1) * 128], identity=identb[:]), reads=["lo", "identb"], writes=["psT"])
            k.op("act", lambda e: e.activation(out=tmp[:], in_=psT[:, 0:1024], func=AF.Copy), reads=["psT"], writes=["tmp"])
            k.op("dve", lambda e: e.tensor_tensor(out=h2f[:], in0=psT[:, 1024:2048], in1=tmp[:], op=ALU.add), reads=["psT", "tmp"], writes=["h2f"])
            k.op("dve", lambda e, mi=mi: e.tensor_tensor(out=tmp[:], in0=h2f[:], in1=mods[mi][:, 2048:3072], op=ALU.mult), reads=["h2f", "mod"], writes=["tmp"])
            k.op("pool", lambda e, s=s: e.tensor_tensor(out=xt[s][:], in0=tmp[:], in1=xt[s][:], op=ALU.add), reads=["tmp", xk], writes=[xk])
            if final:
                c2 = NTILES + t
                k.op("dve", lambda e, s=s: e.tensor_tensor(out=tmp[:], in0=xt[s][:], in1=xt[s][:], op=ALU.mult), reads=[xk], writes=["tmp"])
                k.op("dve", lambda e, c2=c2: e.tensor_reduce(out=ss[:, c2:c2 + 1], in_=tmp[:], axis=AX.X, op=ALU.add), reads=["tmp"], writes=["ss"])
                k.op("act", lambda e, c2=c2: e.activation(out=rstd[:, c2:c2 + 1], in_=ss[:, c2:c2 + 1], func=AF.Sqrt, bias=epsn[:], scale=1.0 / D), reads=["ss", "eps"], writes=["rstd"])
                k.op("dve", lambda e, c2=c2: e.reciprocal(out=rstd[:, c2:c2 + 1], in_=rstd[:, c2:c2 + 1]), reads=["rstd"], writes=["rstd"])
                k.op("dve", lambda e, s=s, c2=c2: e.scalar_tensor_tensor(out=h2f[:], in0=xt[s][:], scalar=rstd[:, c2:c2 + 1], in1=gfin[:], op0=ALU.mult, op1=ALU.mult),
                     reads=[xk, "rstd", "gfin"], writes=["h2f"])
                k.op("sp", lambda e, t=t: e.dma_start(out=xn[t * 128:(t + 1) * 128, :], in_=h2f[:]), reads=["h2f"], dma_key="h2fo")
            k.op("sp", lambda e, s=s, t=t: e.dma_start(out=xo[t * 128:(t + 1) * 128, :], in_=xt[s][:]), reads=[xk], dma_key=xk + "o")
    return k


def moe_weight_shards(inputs, l):
    wgu = np.asarray(inputs["w_gate_up"][l], np.float32)
    wdn = np.asarray(inputs["w_down"][l], np.float32)
    shards = []
    for c in range(NCORES):
        units = np.empty((4, 24, 128, 1024), np.float32)
        for el in range(4):
            e = 4 * c + el
            w = wgu[e].reshape(8, 128, 2048)
            for j in range(8):
                cols = np.concatenate([w[:, :, j * 128:(j + 1) * 128], w[:, :, 1024 + j * 128:1024 + (j + 1) * 128]], axis=2)
                for h2 in range(2):
                    units[el, j * 2 + h2] = cols[h2 * 4:(h2 + 1) * 4].transpose(1, 0, 2).reshape(128, 1024)
            d = wdn[e].reshape(8, 128, 1024)
            for dc in range(8):
                units[el, 16 + dc] = d[:, :, dc * 128:(dc + 1) * 128].transpose(1, 0, 2).reshape(128, 1024)
        shards.append(np.ascontiguousarray(units.reshape(96, 128, 1024)))
    return shards


def common_C(inputs, l):
    bgu = np.asarray(inputs["b_gate_up"][l], np.float32).reshape(32, 16, 128).transpose(2, 0, 1).reshape(128, 512)
    return dict(
        csil=csil_layout(inputs["c"], inputs["c_ctx"]),
        wmod=np.ascontiguousarray(inputs["w_mod"][l][:, 3072:6144]),
        bmod=rep128(inputs["b_mod"][l][3072:6144]),
        g2=rep128(inputs["g_norm2"][l]),
        wr=np.ascontiguousarray(np.asarray(inputs["w_router"][l], np.float32).reshape(8, 128, 32).transpose(1, 0, 2).reshape(128, 256)),
        br=rep128(inputs["b_router"][l]),
        bgu=np.ascontiguousarray(bgu),
        bd=np.ascontiguousarray(np.asarray(inputs["b_down"][l], np.float32)),
        gfin=rep128(inputs["g_final"]),
    )


def dft_consts():
    m = np.arange(64)
    ang = 2.0 * np.pi * np.outer(m, m) / 64.0
    c64 = np.cos(ang).astype(np.float32)
    s64 = np.sin(ang).astype(np.float32)
    z = np.zeros((64, 64), np.float32)
    dftc = np.block([[c64, z], [z, c64]]).astype(np.float32)
    dfts = np.block([[s64, z], [z, s64]]).astype(np.float32)
    return np.ascontiguousarray(dftc), np.ascontiguousarray(dfts)


def dft_ab_table():
    t = np.arange(128, dtype=np.float64)[:, None]
    i = (np.arange(512) % 128).astype(np.float64)[None, :]
    ang = 2.0 * np.pi * t * i / 128.0
    ab = np.concatenate([np.cos(ang), np.sin(ang)], axis=1).astype(np.float32).astype(NPBF)
    return np.ascontiguousarray(ab.reshape(32, 4 * 1024))


def dft_base_core(c):
    p = np.arange(128, dtype=np.float64)[:, None]
    out = np.zeros((128, 4, 1024), np.float32)
    for kb in range(4):
        kk = (c * TLOC + kb * 512 + np.arange(512)).astype(np.float64)[None, :]
        ang = 2.0 * np.pi * p * kk / SEQ
        out[:, kb, 0:512] = np.cos(ang)
        out[:, kb, 512:1024] = np.sin(ang)
    return np.ascontiguousarray(out.reshape(128, 4096).astype(NPBF))


def dft_tables_ctx():
    n = np.arange(CTX)
    idx = (n[:, None] * n[None, :]) % CTX
    cvec = np.cos(2.0 * np.pi * n / CTX).astype(np.float32).astype(NPBF)
    svec = np.sin(2.0 * np.pi * n / CTX).astype(np.float32).astype(NPBF)
    out = []
    for vec in (cvec, svec):
        t = vec[idx].reshape(2, 128, 256).transpose(1, 0, 2).reshape(128, 512)
        out.append(np.ascontiguousarray(t))
    return out


def gather_kv(resA):
    qkT = [np.asarray(r["qkT"]) for r in resA]
    vf = [np.asarray(r["vf"]) for r in resA]
    kall = np.concatenate([qkT[0][3:6, :, 0:CTX]] + [q[3:6, :, CTX:] for q in qkT], axis=2)
    g9 = np.concatenate([qkT[0][9, :, 0:CTX]] + [q[9, :, CTX:] for q in qkT], axis=1)
    kTg = np.stack([np.concatenate([g9[64 * g:64 * g + 64]] * 2, axis=0) for g in range(2)], axis=0)
    vall = np.concatenate([vf[0][0:CTX]] + [v[CTX:] for v in vf], axis=0)
    ones = np.ones((NKEYS, 1), dtype=vall.dtype)
    vaug = []
    for h in range(8):
        va = np.concatenate([vall[:, h * 64:(h + 1) * 64], ones], axis=1)
        vaug.append(va.reshape(NKT, 128, 65).transpose(1, 0, 2).reshape(128, NKT * 65))
    vaug = np.stack(vaug, axis=0)
    f_lat = vall[CTX:, 512:768]
    fall = f_lat.reshape(32, 4, 128, 256).transpose(0, 2, 1, 3).reshape(32, 128, 4 * 256)
    f_ctx = vall[0:CTX, 512:768].reshape(2, 128, 256).transpose(1, 0, 2).reshape(128, 512)
    return dict(kTd=np.ascontiguousarray(kall), kTg=np.ascontiguousarray(kTg), vaug=np.ascontiguousarray(vaug),
                fall=np.ascontiguousarray(fall), fctx=np.ascontiguousarray(f_ctx))


def common_B(inputs, l):
    dftc, dfts = dft_consts()
    tcc, tsc = dft_tables_ctx()
    lam = np.concatenate([inputs["lambda_q1"][l], inputs["lambda_k1"][l], inputs["lambda_q2"][l], inputs["lambda_k2"][l]])
    return dict(
        csil=csil_layout(inputs["c"], inputs["c_ctx"]),
        wmod=np.ascontiguousarray(inputs["w_mod"][l][:, 2048:3072]),
        bmod=rep128(inputs["b_mod"][l][2048:3072]),
        wout=np.ascontiguousarray(inputs["w_out"][l]),
        lam=rep128(lam),
        gsub=np.ascontiguousarray(np.tile(np.asarray(inputs["g_subln"][l], np.float32), 2).reshape(128, 1)),
        wf=np.ascontiguousarray(np.asarray(inputs["w_fnet"][l], np.float32).reshape(2, 128, 64)),
        dftc=dftc, dfts=dfts, tabCc=tcc, tabSc=tsc, ab=dft_ab_table(),
    )


_PROGS = {}


def _prog(name, fn):
    if name not in _PROGS:
        _PROGS[name] = fn().finish()
    return _PROGS[name]


def run_B(resA, shared, xs, xc, inputs, l):
    nc = _prog("B%d" % l, lambda: build_B(l, l == 0))
    cm = common_B(inputs, l)
    in_maps = []
    for c in range(NCORES):
        m = dict(shared)
        m.update(cm)
        m["dbase"] = dft_base_core(c)
        m["qT"] = np.ascontiguousarray(np.asarray(resA[c]["qkT"])[[0, 1, 2, 6, 7, 8]])
        m["xin"] = np.ascontiguousarray(np.concatenate([xc, xs[c]], axis=0))
        in_maps.append(m)
    return run_bass_kernel_spmd(nc, in_maps, core_ids=list(range(NCORES))).results


def run_C(x1s, inputs, l, final):
    full = True
    nc = _prog("Cfull" if full else "C", lambda: build_C(True, dict(full=full)))
    cm = common_C(inputs, l)
    sh = moe_weight_shards(inputs, l)
    if full:
        allw = np.ascontiguousarray(np.concatenate(sh, axis=0))
        del sh
    in_maps = []
    for c in range(NCORES):
        m = dict(cm)
        m["x1"] = np.ascontiguousarray(x1s[c])
        m["wsh"] = allw if full else sh[c]
        in_maps.append(m)
    return run_bass_kernel_spmd(nc, in_maps, core_ids=list(range(NCORES))).results


def kernel(**inputs):
    inputs = {k: np.asarray(v) for k, v in inputs.items()}
    x = inputs["x"][0].astype(np.float32)
    xc = inputs["ctx"][0].astype(np.float32)
    xs = [np.ascontiguousarray(x[c * TLOC:(c + 1) * TLOC]) for c in range(NCORES)]
    rope = rope_tables()
    for l in range(DEPTH):
        resA = run_A(xs, xc, inputs, l, rope)
        shared = gather_kv(resA)
        resB = run_B(resA, shared, xs, xc, inputs, l)
        del shared
        print("[kernel] layer %d: A,B done; launching C" % l, flush=True)
        resC = run_C([np.asarray(r["x1"]) for r in resB], inputs, l, final=(l == DEPTH - 1))
        print("[kernel] layer %d: C done" % l, flush=True)
        xo = [np.asarray(r["xn" if l == DEPTH - 1 else "xo"]) for r in resC]
        xc = np.ascontiguousarray(xo[0][0:CTX])
        xs = [np.ascontiguousarray(o[CTX:]) for o in xo]
    return np.concatenate(xs, axis=0)[None].astype(np.float32)
```
